# Optimizing a Trainium2 kernel written in Bass

```python
import math
import jax
import jax.numpy as jnp
from jax import lax
import numpy as np

D_MODEL = 2048
BATCH = 2
SEQ = 8192
DEPTH = 1

D_SSM = D_MODEL // 2
SSM_GROUP = 16
N_SSM_GROUPS = D_SSM // SSM_GROUP
SSM_STATE = 64
D_ATT = D_MODEL - D_SSM
N_HEADS = 16
N_KV_HEADS = 4
HEAD_DIM = D_ATT // N_HEADS
GQA = N_HEADS // N_KV_HEADS
KV_DIM = N_KV_HEADS * HEAD_DIM
ROT_DIM = HEAD_DIM // 4
ROPE_THETA = 500000.0
CMP_BLOCK = 32
CMP_STRIDE = 16
CMP_HIDDEN = 2 * HEAD_DIM
SEL_BLOCK = 64
N_SELECT = 16
WINDOW = 512
Q_BLOCK = 128
D_FF = 5632
CONV_WIDTH = 3
NORM_EPS = 1e-6
NEG_BIG = -1e30
DT_MIN = 1e-3
DT_MAX = 1e-1
IN_SPLITS = (D_SSM, D_ATT, KV_DIM, KV_DIM, KV_DIM, KV_DIM, KV_DIM, KV_DIM, 3 * N_HEADS)
D_IN = sum(IN_SPLITS)

kernel_name = 'hymba_s5_nsa_convffn_block'


def rmsnorm(x, g):
    xf = x.astype(jnp.float32)
    y = xf * lax.rsqrt(jnp.mean(xf * xf, axis=-1, keepdims=True) + NORM_EPS)
    return (y * g.astype(jnp.float32)).astype(x.dtype)


def modulate(h, shift, scale):
    return h * (1.0 + scale[:, None, :]) + shift[:, None, :]


def partial_rope(x, pos):
    half = ROT_DIM // 2
    inv = ROPE_THETA ** (-jnp.arange(half, dtype=jnp.float32) / half)
    ang = pos.astype(jnp.float32)[:, None] * inv[None, :]
    cos = jnp.cos(ang)[None, :, None, :]
    sin = jnp.sin(ang)[None, :, None, :]
    xr = x[..., :ROT_DIM].astype(jnp.float32)
    x1, x2 = xr[..., :half], xr[..., half:]
    rot = jnp.concatenate([x1 * cos - x2 * sin, x1 * sin + x2 * cos], axis=-1).astype(x.dtype)
    return jnp.concatenate([rot, x[..., ROT_DIM:]], axis=-1)


def _lin_rec_combine(left, right):
    a_l, b_l = left
    a_r, b_r = right
    return a_r * a_l, a_r * b_l + b_r


def s5_mixer(u, lam_re, lam_im, log_dt, b_re, b_im, c_re, c_im, d_skip, w_glu, b_glu):
    bsz, seq, _ = u.shape
    f32 = jnp.float32
    uf = u.astype(f32).reshape(bsz, seq, N_SSM_GROUPS, SSM_GROUP)
    lr = lam_re.astype(f32)
    li = lam_im.astype(f32)
    dt = jnp.exp(log_dt.astype(f32))[:, None]
    mag = jnp.exp(lr * dt)
    ab_re = mag * jnp.cos(li * dt)
    ab_im = mag * jnp.sin(li * dt)
    den = lr * lr + li * li
    num_re = ab_re - 1.0
    f_re = (num_re * lr + ab_im * li) / den
    f_im = (ab_im * lr - num_re * li) / den
    br = b_re.astype(f32)
    bi = b_im.astype(f32)
    bb_re = f_re[..., None] * br - f_im[..., None] * bi
    bb_im = f_re[..., None] * bi + f_im[..., None] * br
    bu = lax.complex(jnp.einsum('bsgh,gph->sbgp', uf, bb_re),
                     jnp.einsum('bsgh,gph->sbgp', uf, bb_im))
    a = jnp.broadcast_to(lax.complex(ab_re, ab_im)[None, None], (seq, 1, N_SSM_GROUPS, SSM_STATE))
    _, states = lax.associative_scan(_lin_rec_combine, (a, bu), axis=0)
    y = (jnp.einsum('sbgp,ghp->bsgh', jnp.real(states), c_re.astype(f32))
         - jnp.einsum('sbgp,ghp->bsgh', jnp.imag(states), c_im.astype(f32))
         + d_skip.astype(f32) * uf)
    y = y.reshape(bsz, seq, D_SSM)
    z = jax.nn.gelu(y)
    out = z * jax.nn.sigmoid(z @ w_glu.astype(f32) + b_glu.astype(f32))
    return out.astype(u.dtype)


def compress_tokens(kv, pe, w1, w2):
    bsz, seq = kv.shape[:2]
    n_cmp = (seq - CMP_BLOCK) // CMP_STRIDE + 1
    idx = jnp.arange(n_cmp)[:, None] * CMP_STRIDE + jnp.arange(CMP_BLOCK)[None, :]
    blocks = kv[:, idx] + pe[None, None, :, None, :]
    flat = blocks.transpose(0, 1, 3, 2, 4).reshape(bsz, n_cmp, N_KV_HEADS, CMP_BLOCK * HEAD_DIM)
    return jax.nn.gelu(flat @ w1) @ w2


def nsa_mixer(q, kc_tok, vc_tok, ks, vs, kw, vw, gate_logits, pe_k, w1_k, w2_k, pe_v, w1_v, w2_v):
    f32 = jnp.float32
    bsz, seq = q.shape[:2]
    dtype = q.dtype
    n_cmp = (seq - CMP_BLOCK) // CMP_STRIDE + 1
    n_sel_blocks = seq // SEL_BLOCK
    n_sel = min(N_SELECT, n_sel_blocks)
    ratio = SEL_BLOCK // CMP_STRIDE
    n_overlap = CMP_BLOCK // CMP_STRIDE
    pad_sel = n_sel_blocks * ratio + n_overlap - 1 - n_cmp
    scale = HEAD_DIM ** -0.5

    kc = compress_tokens(kc_tok, pe_k, w1_k, w2_k)
    vc = compress_tokens(vc_tok, pe_v, w1_v, w2_v)
    cmp_end = jnp.arange(n_cmp) * CMP_STRIDE + CMP_BLOCK - 1
    ksb = ks.reshape(bsz, n_sel_blocks, SEL_BLOCK, N_KV_HEADS, HEAD_DIM).transpose(0, 3, 1, 2, 4)
    vsb = vs.reshape(bsz, n_sel_blocks, SEL_BLOCK, N_KV_HEADS, HEAD_DIM).transpose(0, 3, 1, 2, 4)
    kw_pad = jnp.pad(kw, ((0, 0), (WINDOW, 0), (0, 0), (0, 0)))
    vw_pad = jnp.pad(vw, ((0, 0), (WINDOW, 0), (0, 0), (0, 0)))
    gates = jax.nn.sigmoid(gate_logits.astype(f32)).reshape(bsz, seq, N_KV_HEADS, GQA, 3)
    qg = q.reshape(bsz, seq, N_KV_HEADS, GQA, HEAD_DIM)
    gather_blocks = jax.vmap(jax.vmap(lambda kb, ib: kb[ib]))
    blk = jnp.arange(n_sel_blocks)

    def query_block(i):
        start = i * Q_BLOCK
        t = start + jnp.arange(Q_BLOCK)
        qb = lax.dynamic_slice_in_dim(qg, start, Q_BLOCK, axis=1)
        gb = lax.dynamic_slice_in_dim(gates, start, Q_BLOCK, axis=1)
        s = jnp.einsum('bqhgd,bnhd->bhgqn', qb, kc).astype(f32) * scale
        m = cmp_end[None, :] <= t[:, None]
        p = jax.nn.softmax(jnp.where(m, s, NEG_BIG), axis=-1) * m
        o_cmp = jnp.einsum('bhgqn,bnhd->bqhgd', p.astype(dtype), vc)
        imp = jnp.pad(p.sum(axis=2), ((0, 0), (0, 0), (0, 0), (0, pad_sel)))
        imp = sum(imp[..., o:o + n_sel_blocks * ratio] for o in range(n_overlap))
        imp = imp.reshape(bsz, N_KV_HEADS, Q_BLOCK, n_sel_blocks, ratio).sum(-1)
        cur = t // SEL_BLOCK
        valid = blk[None, :] * SEL_BLOCK <= t[:, None]
        forced = (blk[None, :] == 0) | (blk[None, :] == cur[:, None]) | (blk[None, :] == cur[:, None] - 1)
        imp = jnp.where(forced, -NEG_BIG, jnp.where(valid, imp, NEG_BIG))
        _, sel = lax.top_k(imp, n_sel)
        k_sel = gather_blocks(ksb, sel)
        v_sel = gather_blocks(vsb, sel)
        s = jnp.einsum('bqhgd,bhqnsd->bhgqns', qb, k_sel).astype(f32) * scale
        pos = sel[..., None] * SEL_BLOCK + jnp.arange(SEL_BLOCK)
        m = (pos <= t[None, None, :, None, None])[:, :, None]
        s = jnp.where(m, s, NEG_BIG).reshape(bsz, N_KV_HEADS, GQA, Q_BLOCK, n_sel * SEL_BLOCK)
        p = jax.nn.softmax(s, axis=-1).reshape(bsz, N_KV_HEADS, GQA, Q_BLOCK, n_sel, SEL_BLOCK)
        o_sel = jnp.einsum('bhgqns,bhqnsd->bqhgd', p.astype(dtype), v_sel)
        kwb = lax.dynamic_slice_in_dim(kw_pad, start, WINDOW + Q_BLOCK, axis=1)
        vwb = lax.dynamic_slice_in_dim(vw_pad, start, WINDOW + Q_BLOCK, axis=1)
        kp = start - WINDOW + jnp.arange(WINDOW + Q_BLOCK)
        m = (kp[None, :] <= t[:, None]) & (kp[None, :] > t[:, None] - WINDOW) & (kp[None, :] >= 0)
        s = jnp.einsum('bqhgd,bkhd->bhgqk', qb, kwb).astype(f32) * scale
        p = jax.nn.softmax(jnp.where(m, s, NEG_BIG), axis=-1)
        o_win = jnp.einsum('bhgqk,bkhd->bqhgd', p.astype(dtype), vwb)
        o = gb[..., 0:1] * o_cmp + gb[..., 1:2] * o_sel + gb[..., 2:3] * o_win
        return o.astype(dtype)

    out = lax.map(query_block, jnp.arange(seq // Q_BLOCK))
    return out.transpose(1, 0, 2, 3, 4, 5).reshape(bsz, seq, D_ATT)


def conv_ffn(h, w_up, conv_w, conv_b, w_down):
    up = h @ w_up
    ch = up.shape[-1]
    up = lax.conv_general_dilated(up, conv_w[:, None, :].astype(up.dtype), window_strides=(1,),
                                  padding=[(CONV_WIDTH - 1, 0)],
                                  dimension_numbers=('NWC', 'WIO', 'NWC'),
                                  feature_group_count=ch) + conv_b
    val, gate = jnp.split(up, 2, axis=-1)
    return (jax.nn.silu(gate) * val) @ w_down


def setup_inputs(seed: int = 0) -> dict:
    key = jax.random.key(seed)
    keys = iter(jax.random.split(key, 40))
    f32 = jnp.float32

    def nrm(shape, s):
        return jax.random.normal(next(keys), shape, f32) * s

    L = DEPTH
    G, P, H = N_SSM_GROUPS, SSM_STATE, SSM_GROUP
    inp = {}
    inp['x'] = nrm((BATCH, SEQ, D_MODEL), 1.0)
    inp['c'] = nrm((BATCH, D_MODEL), 1.0)
    inp['w_ada'] = nrm((L, D_MODEL, 6 * D_MODEL), 0.5 * D_MODEL ** -0.5)
    inp['b_ada'] = nrm((L, 6 * D_MODEL), 0.02)
    inp['g_mix_norm'] = 1.0 + nrm((L, D_MODEL), 0.02)
    inp['w_in'] = nrm((L, D_MODEL, D_IN), D_MODEL ** -0.5)
    inp['lam_re'] = -0.5 + nrm((L, G, P), 0.01)
    inp['lam_im'] = jnp.broadcast_to(jnp.pi * jnp.arange(P, dtype=f32), (L, G, P)) + nrm((L, G, P), 0.001)
    inp['log_dt'] = jax.random.uniform(next(keys), (L, G), f32, math.log(DT_MIN), math.log(DT_MAX))
    inp['b_re'] = nrm((L, G, P, H), (2 * H) ** -0.5)
    inp['b_im'] = nrm((L, G, P, H), (2 * H) ** -0.5)
    inp['c_re'] = nrm((L, G, H, P), 0.5)
    inp['c_im'] = nrm((L, G, H, P), 0.5)
    inp['d_skip'] = nrm((L, G, H), 1.0)
    inp['w_glu'] = nrm((L, D_SSM, D_SSM), D_SSM ** -0.5)
    inp['b_glu'] = nrm((L, D_SSM), 0.02)
    inp['pe_k'] = nrm((L, CMP_BLOCK, HEAD_DIM), 0.1)
    inp['w1_k'] = nrm((L, CMP_BLOCK * HEAD_DIM, CMP_HIDDEN), (CMP_BLOCK * HEAD_DIM) ** -0.5)
    inp['w2_k'] = nrm((L, CMP_HIDDEN, HEAD_DIM), CMP_HIDDEN ** -0.5)
    inp['pe_v'] = nrm((L, CMP_BLOCK, HEAD_DIM), 0.1)
    inp['w1_v'] = nrm((L, CMP_BLOCK * HEAD_DIM, CMP_HIDDEN), (CMP_BLOCK * HEAD_DIM) ** -0.5)
    inp['w2_v'] = nrm((L, CMP_HIDDEN, HEAD_DIM), CMP_HIDDEN ** -0.5)
    inp['g_ssm_out'] = 1.0 + nrm((L, D_SSM), 0.02)
    inp['g_nsa_out'] = 1.0 + nrm((L, D_ATT), 0.02)
    inp['w_out'] = nrm((L, D_MODEL, D_MODEL), D_MODEL ** -0.5)
    inp['g_ffn_norm'] = 1.0 + nrm((L, D_MODEL), 0.02)
    inp['w_up'] = nrm((L, D_MODEL, 2 * D_FF), D_MODEL ** -0.5)
    inp['conv_w'] = nrm((L, CONV_WIDTH, 2 * D_FF), CONV_WIDTH ** -0.5)
    inp['conv_b'] = nrm((L, 2 * D_FF), 0.02)
    inp['w_down'] = nrm((L, D_FF, D_MODEL), D_FF ** -0.5)
    inp['g_final'] = 1.0 + nrm((D_MODEL,), 0.02)
    return inp


def reference(x, c, w_ada, b_ada, g_mix_norm, w_in, lam_re, lam_im, log_dt, b_re, b_im, c_re, c_im,
              d_skip, w_glu, b_glu, pe_k, w1_k, w2_k, pe_v, w1_v, w2_v, g_ssm_out, g_nsa_out, w_out,
              g_ffn_norm, w_up, conv_w, conv_b, w_down, g_final):
    bsz, seq, _ = x.shape
    pos = jnp.arange(seq)
    split_at = [int(v) for v in np.cumsum(IN_SPLITS)[:-1]]
    for l in range(DEPTH):
        mod = jax.nn.silu(c) @ w_ada[l] + b_ada[l]
        sh1, sc1, ga1, sh2, sc2, ga2 = jnp.split(mod, 6, axis=-1)
        h = modulate(rmsnorm(x, g_mix_norm[l]), sh1, sc1)
        proj = h @ w_in[l]
        u, q, kc, vc, ks, vs, kw, vw, gl = jnp.split(proj, split_at, axis=-1)
        y_ssm = s5_mixer(u, lam_re[l], lam_im[l], log_dt[l], b_re[l], b_im[l], c_re[l], c_im[l],
                         d_skip[l], w_glu[l], b_glu[l])
        kv_shape = (bsz, seq, N_KV_HEADS, HEAD_DIM)
        q = partial_rope(q.reshape(bsz, seq, N_HEADS, HEAD_DIM), pos)
        kc = partial_rope(kc.reshape(kv_shape), pos)
        ks = partial_rope(ks.reshape(kv_shape), pos)
        kw = partial_rope(kw.reshape(kv_shape), pos)
        y_att = nsa_mixer(q, kc, vc.reshape(kv_shape), ks, vs.reshape(kv_shape), kw, vw.reshape(kv_shape),
                          gl, pe_k[l], w1_k[l], w2_k[l], pe_v[l], w1_v[l], w2_v[l])
        y = jnp.concatenate([rmsnorm(y_ssm, g_ssm_out[l]), rmsnorm(y_att, g_nsa_out[l])], axis=-1) @ w_out[l]
        x = x + ga1[:, None, :] * y
        h = modulate(rmsnorm(x, g_ffn_norm[l]), sh2, sc2)
        x = x + ga2[:, None, :] * conv_ffn(h, w_up[l], conv_w[l], conv_b[l], w_down[l])
    return rmsnorm(x, g_final)
```

```python
import contextlib
import numpy as np
import ml_dtypes
import concourse.bass as bass
import concourse.mybir as mybir
from concourse.bass_utils import run_bass_kernel_spmd

F32 = mybir.dt.float32
BF16 = mybir.dt.bfloat16
I32 = mybir.dt.int32
AF = mybir.ActivationFunctionType
ALU = mybir.AluOpType
NPBF = ml_dtypes.bfloat16

D = 2048
S = 8192
NT = 64
EPS = 1e-6
PI = float(np.pi)
TWO_PI = float(2 * np.pi)

DEBUG = {}


PSUM_KEYS = set(['p0_psm', 'p0_pstm', 'a0_ptr0', 'a0_ptr1', 'psu0', 'psu1', 'psb0', 'psb1', 'psy', 's5_pst', 'a2_psf0', 'a2_psf1', 'a2_psr', 'a2_pst0', 'a2_pst1', 'b_pss0', 'b_pss1', 'b_psm', 'b_oacc', 'b_psimp', 'b_pTA', 'b_pTB', 'b_pmb', 'p2_psg0', 'p2_psg1', 'p2_psA', 'p2_psB', 'p2_psst', 'p2_ptr0', 'p2_ptr1', 'p3_psu0', 'p3_psu1', 'p3_psu2', 'p3_psd0', 'p3_psd1'])


class Sched:
    EPOCH = 20000

    def __init__(self, nc, stack):
        self.nc = nc
        self.stack = stack
        self.engs = {"pe": nc.tensor, "act": nc.scalar, "dve": nc.vector, "pool": nc.gpsimd, "sp": nc.sync}
        self.cnt = {e: 0 for e in self.engs}
        self.esem = {e: None for e in self.engs}
        self.waited = {e: {} for e in self.engs}
        self.last_w = {}
        self.readers = {}
        self.chan = {}
        self.nsem = 0
        self.ninstr = 0

    def _newsem(self, name):
        self.nsem += 1
        return self.stack.enter_context(self.nc.semaphore(f"s{self.nsem}_{name}"))

    def _wait(self, eng, tok):
        if tok is None:
            return
        sem, val, src = tok
        w = self.waited[eng]
        k = id(sem)
        if w.get(k, 0) >= val:
            return
        w[k] = val
        self.engs[eng].wait_ge(sem, val)

    def op(self, eng, fn, reads=(), writes=(), chan=None, pe_chain=False):
        writes = list(writes) + [k for k in reads if isinstance(k, str) and k in PSUM_KEYS and k not in writes]
        toks = []
        for k in reads:
            t = self.last_w.get(k)
            if t is not None:
                toks.append(t)
        for k in writes:
            t = self.last_w.get(k)
            if t is not None:
                toks.append(t)
            toks.extend(self.readers.get(k, {}).values())
        for t in toks:
            if pe_chain and t[2] == "pe" and eng == "pe":
                continue
            self._wait(eng, t)
        e = self.engs[eng]
        if chan is None:
            if self.esem[eng] is None or self.cnt[eng] >= self.EPOCH:
                self.esem[eng] = self._newsem(eng)
                self.cnt[eng] = 0
            ins = fn(e)
            self.cnt[eng] += 1
            ins.then_inc(self.esem[eng], 1)
            tok = (self.esem[eng], self.cnt[eng], eng)
        else:
            if chan not in self.chan:
                self.chan[chan] = [self._newsem(chan), 0]
            c = self.chan[chan]
            inss = fn(e)
            if not isinstance(inss, (list, tuple)):
                inss = [inss]
            for ins in inss:
                ins.then_inc(c[0], 16)
                c[1] += 16
            tok = (c[0], c[1], "dma")
        self.ninstr += 1
        for k in writes:
            self.last_w[k] = tok
            self.readers[k] = {}
        for k in reads:
            if k not in writes:
                r = self.readers.setdefault(k, {})
                old = r.get(id(tok[0]))
                if old is None or old[1] < tok[1]:
                    r[id(tok[0])] = tok
        return tok

    def cc(self, fn, reads=(), writes=(), name="cc"):
        eng = "pool"
        toks = []
        for k in reads:
            t = self.last_w.get(k)
            if t is not None:
                toks.append(t)
        for k in writes:
            t = self.last_w.get(k)
            if t is not None:
                toks.append(t)
            toks.extend(self.readers.get(k, {}).values())
        for t in toks:
            self._wait(eng, t)
        sem = self._newsem(name)
        ins = fn(self.engs[eng])
        ins.then_inc(sem, 1)
        tok = (sem, 1, "cc")
        for k in writes:
            self.last_w[k] = tok
            self.readers[k] = {}
        for k in reads:
            if k not in writes:
                self.readers.setdefault(k, {})[id(sem)] = tok
        return tok

    def barrier(self):
        toks = []
        for e in self.engs:
            if self.esem[e] is not None and self.cnt[e] > 0:
                toks.append((self.esem[e], self.cnt[e], e))
        for ch, c in self.chan.items():
            if c[1] > 0:
                toks.append((c[0], c[1], "dma"))
        for e in self.engs:
            for t in toks:
                self._wait(e, t)

    def final_wait(self, eng, keys):
        for k in keys:
            self._wait(eng, self.last_w.get(k))


class Ctx:
    pass


QT0 = 44
QF = 47
NQ = 17
ZC0 = 6142
SPEC = {}


def _reg(n, shp, dt=F32):
    SPEC[n] = (list(shp), dt)


_reg("x_ctx", [S, D]); _reg("valid", [128, 64]); _reg("x_own", [2050, D]); _reg("c_own", [D])
_reg("w_ada", [D, 12288]); _reg("b_ada", [12288]); _reg("g_mix", [D]); _reg("w_u", [D, 1024])
_reg("w_fm", [4, D, 640]); _reg("w_tm", [4, D, 140])
for _n in ("lam_re_t", "lam_im_t", "log_dt_t"):
    _reg(_n, [4, 128, 8])
for _n in ("b_re_t", "b_im_t"):
    _reg(_n, [4, 128, 8, 16])
for _n in ("c_re_t", "c_im_t"):
    _reg(_n, [4, 128, 8, 64])
_reg("d_t", [4, 128, 2])
for _s in ("k", "v"):
    _reg("pe_" + _s, [32, 64]); _reg("w1_" + _s, [2048, 128]); _reg("w2_" + _s, [128, 64])
_reg("w_glu", [1024, 1024]); _reg("b_glu", [1024]); _reg("g_ssm", [1024]); _reg("g_nsa", [1024])
_reg("w_out", [D, D]); _reg("g_ffn", [D]); _reg("w_up", [D, 11264]); _reg("conv_w", [3, 11264]); _reg("conv_b", [11264])
_reg("w_down", [5632, D]); _reg("g_final", [D])
_reg("ident_bf", [128, 128], BF16); _reg("ident_f", [128, 128]); _reg("iota256", [128, 256])
_reg("ropeC", [128, S]); _reg("ropeS", [128, S]); _reg("ropeCk", [128, S]); _reg("ropeSk", [128, S])
_reg("Pm", [128, 128], BF16); _reg("causal4", [128, 512], BF16); _reg("ones_col", [128, 1], BF16)
_reg("cmpmask", [NQ, 128, 4, 512], BF16); _reg("winmask", [NQ, 128, 5, 512], BF16)
_reg("Wimp", [128, 4, 128], BF16); _reg("Zexp", [128, S], BF16); _reg("selbias", [NQ, 128, 128]); _reg("blockvalid", [128, 128])
_reg("haloflag", [128, 1])
_reg("c_t", [128, 16]); _reg("b_ada_t", [128, 96]); _reg("b_glu_t", [128, 8]); _reg("g_ssm_t", [128, 8]); _reg("g_nsa_t", [128, 8])
_reg("conv_w_t", [128, 88, 3]); _reg("conv_b_t", [128, 88]); _reg("pe_kT", [64, 32]); _reg("pe_vT", [64, 32])


def build(opts=None, dbg=()):
    nc = bass.Bass("TRN2", target_bir_lowering=False)
    K = Ctx()
    K.nc = nc
    K.dbg = dbg
    K.opts = opts or {}
    K.uid = 0

    def din(name, shape, dt=F32):
        return nc.dram_tensor(name, list(shape), dt, kind="ExternalInput").ap()

    def dout(name, shape, dt=F32):
        return nc.dram_tensor(name, list(shape), dt, kind="ExternalOutput").ap()

    def dscr(name, shape, dt=F32):
        return nc.dram_tensor(name, list(shape), dt).ap()

    K.dout, K.dscr = dout, dscr

    class LazyI(dict):
        def __missing__(self, k):
            shp, dt = SPEC[k]
            v = din(k, shp, dt)
            self[k] = v
            return v

    I = LazyI()
    K.I = I
    K.out = dout("out", [2048, D])
    K.dbg_out = {}
    with contextlib.ExitStack() as stack:
        K.stack = stack
        sc = Sched(nc, stack)
        K.sc = sc

        def sb(name, shape, dt=F32, st=None):
            K.uid += 1
            return (st or stack).enter_context(nc.sbuf_tensor(f"sb{K.uid}_{name}", list(shape), dt))

        def ps(name, shape, dt=F32, st=None):
            K.uid += 1
            return (st or stack).enter_context(nc.psum_tensor(f"ps{K.uid}_{name}", list(shape), dt))

        K.sb, K.ps = sb, ps
        K.ident_bf = sb("ident_bf", [128, 128], BF16)
        K.ident_f = sb("ident_f", [128, 128])
        K.ones_col = sb("ones_col", [128, 1], BF16)
        sc.op("sp", lambda e: e.dma_start(out=K.ident_bf[:], in_=I["ident_bf"]), writes=["ident_bf"], chan="c_idb")
        sc.op("sp", lambda e: e.dma_start(out=K.ident_f[:], in_=I["ident_f"]), writes=["ident_f"], chan="c_idf")
        sc.op("sp", lambda e: e.dma_start(out=K.ones_col[:], in_=I["ones_col"]), writes=["ones_col"], chan="c_ones")
        K.modT = sb("modT", [128, 96])
        K.mod_d = dscr("mod_d", [96, 128])
        K.hT_d = dscr("hT_d", [16, 128, 16 * 512], BF16)
        K.z_d = dscr("z_d", [1024, S], BF16)
        K.a_d = dscr("a_d", [1024, NQ * 128], BF16)
        K.xm_d = dscr("xm_d", [2050, D])
        K.h2T_d = dscr("h2T_d", [128, 16, 2064], BF16)
        K.xo_d = dscr("xo_d", [2048, D])
        ph = K.opts.get("phases", ("p0", "a0", "s5", "att", "p2"))
        heads = K.opts.get("heads", (0, 1, 2, 3))
        if "p0" in ph:
            phase0(K)
            sc.barrier()
        if "a0" in ph:
            pass_a0(K)
            sc.barrier()
        if "s5" in ph:
            for hq in heads:
                pass_s5(K, hq)
                sc.barrier()
        if "att" in ph:
            for hq in heads:
                pass_att(K, hq)
                sc.barrier()
        if "p2" in ph:
            phase2a(K)
            sc.barrier()
            phase2b(K)
        outs = ["out0", "out1"] + ["dbg_" + d for d in K.dbg_out]
        sc.final_wait("sp", outs)
    return nc, K


def dbg_dump(K, name, src_ap, shape, dt, reads):
    if name not in K.dbg:
        return
    d = K.dout("dbg_" + name, shape, dt)
    K.dbg_out[name] = d
    K.sc.op("sp", lambda e: e.dma_start(out=d, in_=src_ap), reads=reads, writes=["dbg_" + name], chan="dbg_" + name)


def phase0(K):
    nc, sc, I = K.nc, K.sc, K.I
    with contextlib.ExitStack() as st:
        sb = lambda n, s, d=F32: K.sb(n, s, d, st)
        ps = lambda n, s, d=F32: K.ps(n, s, d, st)
        c_t = sb("p0_c", [128, 16]); scl = sb("p0_sc", [128, 16]); b_t = sb("p0_b", [128, 96])
        wa = [sb(f"p0_wa{i}", [128, 16, 384]) for i in range(2)]
        psm_ = ps("p0_ps", [128, 512]); psm = psm_[:, 0:96]
        sc.op("sp", lambda e: e.dma_start(out=c_t[:], in_=I["c_t"]), writes=["p0_c"], chan="p0_c")
        sc.op("sp", lambda e: e.dma_start(out=b_t[:], in_=I["b_ada_t"]), writes=["p0_b"], chan="p0_b")
        sc.op("act", lambda e: e.activation(out=scl[:], in_=c_t[:], func=AF.Silu), reads=["p0_c"], writes=["p0_sc"])
        wsrc = I["w_ada"].rearrange("(k p) n -> p k n", p=128)
        for g in range(32):
            s = g % 2
            sc.op("sp" if s == 0 else "pool", lambda e, g=g, s=s: e.dma_start(out=wa[s][:], in_=wsrc[:, :, g * 384:(g + 1) * 384]),
                  writes=[f"p0_wa{s}"], chan=f"p0_wa{s}")
            for m3 in range(3):
                m = g * 3 + m3
                for k in range(16):
                    sc.op("pe", lambda e, s=s, m3=m3, m=m, k=k: e.matmul(psm[:, m:m + 1], lhsT=wa[s][:, k, m3 * 128:(m3 + 1) * 128],
                                                                         rhs=scl[:, k:k + 1], start=(k == 0), stop=(k == 15)),
                          reads=[f"p0_wa{s}", "p0_sc"], writes=["p0_psm"], pe_chain=True)
        sc.op("dve", lambda e: e.tensor_tensor(out=K.modT[:], in0=psm[:], in1=b_t[:], op=ALU.add), reads=["p0_psm", "p0_b"], writes=["modT"])
        pstm = ps("p0_pstm", [128, 512]); modrow = sb("p0_modrow", [96, 128])
        sc.op("pe", lambda e: e.transpose(out=pstm[0:96, 0:128], in_=K.modT[:], identity=K.ident_f[:]), reads=["modT", "ident_f"], writes=["p0_pstm"])
        sc.op("act", lambda e: e.copy(out=modrow[:], in_=pstm[0:96, 0:128]), reads=["p0_pstm"], writes=["p0_modrow"])
        sc.op("sp", lambda e: e.dma_start(out=K.mod_d, in_=modrow[:]), reads=["p0_modrow"], writes=["mod_d"], chan="p0_ms")
        dbg_dump(K, "mod", K.modT[:], [128, 96], F32, ["modT"])


def bcast_row(K, dst, which, chan):
    src = K.mod_d[which * 16:(which + 1) * 16, :].rearrange("k p -> (k p)").partition_broadcast(128)
    K.sc.op("sp", lambda e: e.dma_start(out=dst[:], in_=src), reads=["mod_d"], writes=[chan], chan=chan)


def pass_a0(K):
    nc, sc, I = K.nc, K.sc, K.I
    with contextlib.ExitStack() as st:
        sb = lambda n, s, d=F32: K.sb(n, s, d, st)
        ps = lambda n, s, d=F32: K.ps(n, s, d, st)
        gB = sb("a0_gB", [128, D]); scaleB = sb("a0_scaleB", [128, D]); shiftB = sb("a0_shiftB", [128, D])
        valid = sb("a0_valid", [128, 64])
        sc.op("sp", lambda e: e.dma_start(out=gB[:], in_=I["g_mix"].partition_broadcast(128)), writes=["a0_gB"], chan="a0_gB")
        sc.op("sp", lambda e: e.dma_start(out=valid[:], in_=I["valid"]), writes=["a0_valid"], chan="a0_valid")
        bcast_row(K, scaleB, 1, "a0_scaleB")
        bcast_row(K, shiftB, 0, "a0_shiftB")
        sc.op("dve", lambda e: e.scalar_tensor_tensor(out=scaleB[:], in0=scaleB[:], scalar=1.0, in1=gB[:], op0=ALU.add, op1=ALU.mult),
              reads=["a0_scaleB", "a0_gB"], writes=["a0_scaleB"])
        xt = [sb(f"a0_xt{i}", [128, D]) for i in range(2)]
        junk = sb("a0_junk", [128, D], BF16)
        tmp = sb("a0_tmp", [128, D])
        hs_ = [sb(f"a0_hs{i}", [128, D], BF16) for i in range(2)]
        ssq = sb("a0_ssq", [128, 2]); rstd = sb("a0_rstd", [128, 2]); rv = sb("a0_rv", [128, 2])
        hT = [sb(f"a0_hT{i}", [128, 16, 512], BF16) for i in range(2)]
        ptr = [ps(f"a0_ptr{i}", [128, 8, 128], BF16) for i in range(2)]
        xb_t = I["x_ctx"].rearrange("(n p) d -> n p d", p=128)
        for ci in range(K.opts.get("a0_nch", K.opts.get("nch", 16))):
            hb = ci % 2
            for tt in range(4):
                ti = ci * 4 + tt
                s = ti % 2
                sc.op("sp", lambda e, ti=ti, s=s: e.dma_start(out=xt[s][:], in_=xb_t[ti]), writes=[f"a0_xt{s}"], chan=f"a0_xt{s}")
                sc.op("act", lambda e, s=s: e.activation(out=junk[:], in_=xt[s][:], func=AF.Square, accum_out=ssq[:, s:s + 1]),
                      reads=[f"a0_xt{s}"], writes=["a0_junk", f"a0_ssq{s}"])
                sc.op("act", lambda e, s=s: e.activation(out=rstd[:, s:s + 1], in_=ssq[:, s:s + 1], func=AF.Ln, scale=1.0 / D, bias=EPS),
                      reads=[f"a0_ssq{s}"], writes=[f"a0_rstd{s}"])
                sc.op("act", lambda e, s=s: e.activation(out=rstd[:, s:s + 1], in_=rstd[:, s:s + 1], func=AF.Exp, scale=-0.5),
                      reads=[f"a0_rstd{s}"], writes=[f"a0_rstd{s}"])
                sc.op("dve", lambda e, s=s, ti=ti: e.tensor_tensor(out=rv[:, s:s + 1], in0=rstd[:, s:s + 1], in1=valid[:, ti:ti + 1], op=ALU.mult),
                      reads=[f"a0_rstd{s}", "a0_valid"], writes=[f"a0_rv{s}"])
                sc.op("dve", lambda e, s=s: e.scalar_tensor_tensor(out=tmp[:], in0=xt[s][:], scalar=rv[:, s:s + 1], in1=scaleB[:], op0=ALU.mult, op1=ALU.mult),
                      reads=[f"a0_xt{s}", f"a0_rv{s}", "a0_scaleB"], writes=["a0_tmp"])
                sc.op("dve", lambda e, s=s, ti=ti: e.scalar_tensor_tensor(out=hs_[s][:], in0=shiftB[:], scalar=valid[:, ti:ti + 1], in1=tmp[:], op0=ALU.mult, op1=ALU.add),
                      reads=["a0_shiftB", "a0_valid", "a0_tmp"], writes=[f"a0_hs{s}"])
                for half in range(2):
                    for k8 in range(8):
                        k = half * 8 + k8
                        sc.op("pe", lambda e, s=s, k=k, k8=k8, half=half: e.transpose(out=ptr[half][:, k8, :], in_=hs_[s][:, k * 128:(k + 1) * 128], identity=K.ident_bf[:]),
                              reads=[f"a0_hs{s}", "ident_bf"], writes=[f"a0_ptr{half}"], pe_chain=True)
                    sc.op("act", lambda e, half=half, hb=hb, tt=tt: e.copy(out=hT[hb][:, half * 8:(half + 1) * 8, tt * 128:(tt + 1) * 128], in_=ptr[half][:]),
                          reads=[f"a0_ptr{half}"], writes=[f"a0_hT{hb}"])
            sc.op("sp", lambda e, ci=ci, hb=hb: e.dma_start(out=K.hT_d[ci], in_=hT[hb][:].rearrange("p k t -> p (k t)")),
                  reads=[f"a0_hT{hb}"], writes=[("hT_d", ci)], chan=f"a0_hst{hb}")
            if "hT" in K.dbg and ci == K.opts.get("dbg_ci", 15):
                dbg_dump(K, "hT", hT[hb][:].rearrange("p k t -> p (k t)"), [128, 8192], BF16, [f"a0_hT{hb}"])


def sincos(K, sb, ang, n, out_sin, out_cos, tag):
    sc = K.sc
    C1 = 6.28125
    C2 = TWO_PI - 6.28125
    ki = sb(tag + "_ki", [128, n], I32)
    kf = sb(tag + "_kf", [128, n])
    r = sb(tag + "_r", [128, n])
    m = sb(tag + "_m", [128, n])
    for which, outp, shift in (("s", out_sin, 0.0), ("c", out_cos, PI / 2)):
        kk = tag + which
        sc.op("dve", lambda e, shift=shift: e.tensor_scalar(out=kf[:], in0=ang, scalar1=shift, scalar2=1.0 / TWO_PI,
                                                            op0=ALU.add, op1=ALU.mult), reads=[tag + "_ang"], writes=[tag + "_kf"])
        sc.op("dve", lambda e: e.tensor_copy(out=ki[:], in_=kf[:]), reads=[tag + "_kf"], writes=[tag + "_ki"])
        sc.op("dve", lambda e: e.tensor_copy(out=kf[:], in_=ki[:]), reads=[tag + "_ki"], writes=[tag + "_kf"])
        sc.op("dve", lambda e, shift=shift: e.tensor_scalar(out=r[:], in0=ang, scalar1=shift, scalar2=None, op0=ALU.add),
              reads=[tag + "_ang"], writes=[tag + "_r"])
        sc.op("dve", lambda e: e.scalar_tensor_tensor(out=r[:], in0=kf[:], scalar=-C1, in1=r[:], op0=ALU.mult, op1=ALU.add),
              reads=[tag + "_kf", tag + "_r"], writes=[tag + "_r"])
        sc.op("dve", lambda e: e.scalar_tensor_tensor(out=r[:], in0=kf[:], scalar=-C2, in1=r[:], op0=ALU.mult, op1=ALU.add),
              reads=[tag + "_kf", tag + "_r"], writes=[tag + "_r"])
        sc.op("dve", lambda e: e.tensor_scalar(out=m[:], in0=r[:], scalar1=PI, scalar2=-TWO_PI, op0=ALU.is_gt, op1=ALU.mult),
              reads=[tag + "_r"], writes=[tag + "_m"])
        sc.op("dve", lambda e: e.tensor_tensor(out=r[:], in0=r[:], in1=m[:], op=ALU.add),
              reads=[tag + "_r", tag + "_m"], writes=[tag + "_r"])
        sc.op("dve", lambda e: e.tensor_scalar(out=m[:], in0=r[:], scalar1=-PI, scalar2=TWO_PI, op0=ALU.is_lt, op1=ALU.mult),
              reads=[tag + "_r"], writes=[tag + "_m"])
        sc.op("dve", lambda e: e.tensor_tensor(out=r[:], in0=r[:], in1=m[:], op=ALU.add),
              reads=[tag + "_r", tag + "_m"], writes=[tag + "_r"])
        sc.op("dve", lambda e: e.tensor_scalar(out=r[:], in0=r[:], scalar1=-PI, scalar2=PI, op0=ALU.max, op1=ALU.min),
              reads=[tag + "_r"], writes=[tag + "_r"])
        sc.op("act", lambda e, outp=outp: e.activation(out=outp, in_=r[:], func=AF.Sin), reads=[tag + "_r"], writes=[kk])


def pass_s5(K, hq):
    nc, sc, I = K.nc, K.sc, K.I
    PENG = K.opts.get("peng", "pool")
    STOP = K.opts.get("s5_stop", 99)
    with contextlib.ExitStack() as st:
        sb = lambda n, s, d=F32: K.sb(n, s, d, st)
        ps = lambda n, s, d=F32: K.ps(n, s, d, st)
        lr = sb("s5_lr", [128, 8]); li = sb("s5_li", [128, 8]); ldt = sb("s5_ldt", [128, 8])
        for t_, n_ in ((lr, "lam_re_t"), (li, "lam_im_t"), (ldt, "log_dt_t")):
            sc.op("sp", lambda e, t_=t_, n_=n_: e.dma_start(out=t_[:], in_=I[n_][hq]), writes=["s5_" + n_], chan="s5_" + n_)
        bre = sb("s5_bre", [128, 8, 16]); bim = sb("s5_bim", [128, 8, 16])
        cre = sb("s5_cre", [128, 8, 64]); cim = sb("s5_cim", [128, 8, 64])
        d_t = sb("s5_d", [128, 2])
        for t_, n_ in ((bre, "b_re_t"), (bim, "b_im_t"), (cre, "c_re_t"), (cim, "c_im_t"), (d_t, "d_t")):
            sc.op("sp", lambda e, t_=t_, n_=n_: e.dma_start(out=t_[:], in_=I[n_][hq]), writes=["s5_" + n_], chan="s5_" + n_)
        iot = sb("s5_iota", [128, 256])
        sc.op("sp", lambda e: e.dma_start(out=iot[:], in_=I["iota256"]), writes=["s5_iota"], chan="s5_iota")
        dt = sb("s5_dt", [128, 8]); mag = sb("s5_mag", [128, 8]); th = sb("s5_th", [128, 8])
        tmp = sb("s5_tmp", [128, 8]); tmp2 = sb("s5_tmp2", [128, 8])
        sn = sb("s5_sn", [128, 8]); cs = sb("s5_cs", [128, 8])
        sc.op("act", lambda e: e.activation(out=dt[:], in_=ldt[:], func=AF.Exp), reads=["s5_log_dt_t"], writes=["s5_dt"])
        sc.op("dve", lambda e: e.tensor_tensor(out=tmp[:], in0=lr[:], in1=dt[:], op=ALU.mult),
              reads=["s5_lam_re_t", "s5_dt"], writes=["s5_tmp"])
        sc.op("act", lambda e: e.activation(out=mag[:], in_=tmp[:], func=AF.Exp), reads=["s5_tmp"], writes=["s5_mag"])
        sc.op("dve", lambda e: e.tensor_tensor(out=th[:], in0=li[:], in1=dt[:], op=ALU.mult),
              reads=["s5_lam_im_t", "s5_dt"], writes=["s5_th_ang"])
        sincos(K, sb, th[:], 8, sn[:], cs[:], "s5_th")
        abre = sb("s5_abre", [128, 8]); abim = sb("s5_abim", [128, 8])
        fre = sb("s5_fre", [128, 8]); fim = sb("s5_fim", [128, 8]); nfim = sb("s5_nfim", [128, 8])
        den = sb("s5_den", [128, 8]); nre = sb("s5_nre", [128, 8])
        sc.op("dve", lambda e: e.tensor_tensor(out=abre[:], in0=mag[:], in1=cs[:], op=ALU.mult), reads=["s5_mag", "s5_thc"], writes=["s5_abre"])
        sc.op("dve", lambda e: e.tensor_tensor(out=abim[:], in0=mag[:], in1=sn[:], op=ALU.mult), reads=["s5_mag", "s5_ths"], writes=["s5_abim"])
        sc.op("dve", lambda e: e.tensor_tensor(out=den[:], in0=lr[:], in1=lr[:], op=ALU.mult), reads=["s5_lam_re_t"], writes=["s5_den"])
        sc.op("dve", lambda e: e.tensor_tensor(out=tmp[:], in0=li[:], in1=li[:], op=ALU.mult), reads=["s5_lam_im_t"], writes=["s5_tmp"])
        sc.op("dve", lambda e: e.tensor_tensor(out=den[:], in0=den[:], in1=tmp[:], op=ALU.add), reads=["s5_den", "s5_tmp"], writes=["s5_den"])
        sc.op("dve", lambda e: e.reciprocal(out=den[:], in_=den[:]), reads=["s5_den"], writes=["s5_den"])
        sc.op("dve", lambda e: e.tensor_scalar(out=nre[:], in0=abre[:], scalar1=-1.0, scalar2=None, op0=ALU.add), reads=["s5_abre"], writes=["s5_nre"])
        sc.op("dve", lambda e: e.tensor_tensor(out=tmp[:], in0=nre[:], in1=lr[:], op=ALU.mult), reads=["s5_nre", "s5_lam_re_t"], writes=["s5_tmp"])
        sc.op("dve", lambda e: e.tensor_tensor(out=tmp2[:], in0=abim[:], in1=li[:], op=ALU.mult), reads=["s5_abim", "s5_lam_im_t"], writes=["s5_tmp2"])
        sc.op("dve", lambda e: e.tensor_tensor(out=tmp[:], in0=tmp[:], in1=tmp2[:], op=ALU.add), reads=["s5_tmp", "s5_tmp2"], writes=["s5_tmp"])
        sc.op("dve", lambda e: e.tensor_tensor(out=fre[:], in0=tmp[:], in1=den[:], op=ALU.mult), reads=["s5_tmp", "s5_den"], writes=["s5_fre"])
        sc.op("dve", lambda e: e.tensor_tensor(out=tmp[:], in0=abim[:], in1=lr[:], op=ALU.mult), reads=["s5_abim", "s5_lam_re_t"], writes=["s5_tmp"])
        sc.op("dve", lambda e: e.tensor_tensor(out=tmp2[:], in0=nre[:], in1=li[:], op=ALU.mult), reads=["s5_nre", "s5_lam_im_t"], writes=["s5_tmp2"])
        sc.op("dve", lambda e: e.tensor_tensor(out=tmp[:], in0=tmp[:], in1=tmp2[:], op=ALU.subtract), reads=["s5_tmp", "s5_tmp2"], writes=["s5_tmp"])
        sc.op("dve", lambda e: e.tensor_tensor(out=fim[:], in0=tmp[:], in1=den[:], op=ALU.mult), reads=["s5_tmp", "s5_den"], writes=["s5_fim"])
        sc.op("dve", lambda e: e.tensor_scalar(out=nfim[:], in0=fim[:], scalar1=-1.0, scalar2=None, op0=ALU.mult), reads=["s5_fim"], writes=["s5_nfim"])
        ang = sb("s5_ang", [128, 8 * 256])
        Ct = sb("s5_Ct", [128, 8, 256]); St = sb("s5_St", [128, 8, 256])
        for k4 in range(8):
            sc.op("dve", lambda e, k4=k4: e.tensor_scalar(out=ang[:, k4 * 256:(k4 + 1) * 256], in0=iot[:], scalar1=th[:, k4:k4 + 1],
                                                          scalar2=None, op0=ALU.mult),
                  reads=["s5_iota", "s5_th_ang"], writes=["s5_tab_ang"])
        sincos(K, sb, ang[:], 8 * 256, St[:].rearrange("p a b -> p (a b)"), Ct[:].rearrange("p a b -> p (a b)"), "s5_tab")
        ang2 = sb("s5_ang2", [128, 8]); c256 = sb("s5_c256", [128, 8]); s256 = sb("s5_s256", [128, 8]); ns256 = sb("s5_ns256", [128, 8])
        sc.op("dve", lambda e: e.tensor_scalar(out=ang2[:], in0=th[:], scalar1=256.0, scalar2=None, op0=ALU.mult),
              reads=["s5_th_ang"], writes=["s5_r256_ang"])
        sincos(K, sb, ang2[:], 8, s256[:], c256[:], "s5_r256")
        sc.op("dve", lambda e: e.tensor_scalar(out=ns256[:], in0=s256[:], scalar1=-1.0, scalar2=None, op0=ALU.mult),
              reads=["s5_r256s"], writes=["s5_ns256"])
        Amag = sb("s5_Amag", [128, 8, 256])
        for k4 in range(8):
            sc.op("dve", lambda e, k4=k4: e.tensor_scalar(out=Amag[:, k4, :], in0=iot[:], scalar1=0.0, scalar2=mag[:, k4:k4 + 1],
                                                          op0=ALU.mult, op1=ALU.add), reads=["s5_iota", "s5_mag"], writes=["s5_Amag"])
        Wtmp = sb("s5_Wtmp", [128, 128])
        BT = sb("s5_BT", [128, 16, 128], BF16)
        pst_ = ps("s5_pst", [128, 512]); pst = pst_[:, 0:128]
        for k4 in range(8):
            for ri in range(2):
                sc.op("dve", lambda e: e.memset(Wtmp[:], 0.0), writes=["s5_Wtmp"])
                for g2 in range(2):
                    gl = (2 * k4 + g2) % 8
                    rows = slice(g2 * 64, g2 * 64 + 64)
                    cols = slice(gl * 16, gl * 16 + 16)
                    if ri == 0:
                        a_, b_, sa, sbn = bre, bim, fre, nfim
                    else:
                        a_, b_, sa, sbn = bim, bre, fre, fim
                    sc.op("dve", lambda e, rows=rows, cols=cols, a_=a_, sa=sa, k4=k4: e.tensor_scalar(
                        out=Wtmp[rows, cols], in0=a_[rows, k4, :], scalar1=sa[rows, k4:k4 + 1], scalar2=None, op0=ALU.mult),
                        reads=["s5_b_re_t", "s5_b_im_t", "s5_fre"], writes=["s5_Wtmp"])
                    sc.op("dve", lambda e, rows=rows, cols=cols, b_=b_, sbn=sbn, k4=k4: e.scalar_tensor_tensor(
                        out=Wtmp[rows, cols], in0=b_[rows, k4, :], scalar=sbn[rows, k4:k4 + 1], in1=Wtmp[rows, cols],
                        op0=ALU.mult, op1=ALU.add),
                        reads=["s5_b_re_t", "s5_b_im_t", "s5_fim", "s5_nfim"], writes=["s5_Wtmp"])
                sc.op("pe", lambda e: e.transpose(out=pst[:], in_=Wtmp[:], identity=K.ident_f[:]),
                      reads=["s5_Wtmp", "ident_f"], writes=["s5_pst"])
                sc.op("act", lambda e, k4=k4, ri=ri: e.copy(out=BT[:, k4 * 2 + ri, :], in_=pst[:]),
                      reads=["s5_pst"], writes=["s5_BT"])
        CTb = sb("s5_CTb", [128, 8, 64], BF16); nCTb = sb("s5_nCTb", [128, 8, 64], BF16)
        sc.op("dve", lambda e: e.tensor_copy(out=CTb[:], in_=cre[:]), reads=["s5_c_re_t"], writes=["s5_CTb"])
        sc.op("dve", lambda e: e.tensor_scalar(out=nCTb[:], in0=cim[:], scalar1=-1.0, scalar2=None, op0=ALU.mult),
              reads=["s5_c_im_t"], writes=["s5_nCTb"])
        ire = sb("s5_ire", [128, 8]); iim = sb("s5_iim", [128, 8]); itmp = sb("s5_itmp", [128, 8])
        sc.op("dve", lambda e: e.memset(ire[:], 0.0), writes=["s5_ire"])
        sc.op("dve", lambda e: e.memset(iim[:], 0.0), writes=["s5_iim"])


        wu = sb("s5_wu", [128, 16, 256], BF16)
        sc.op("pool", lambda e: e.dma_start(out=wu[:], in_=I["w_u"].rearrange("(k p) n -> p k n", p=128)[:, :, hq * 256:(hq + 1) * 256]),
              writes=["s5_wu"], chan="s5_wu")
        hT = [sb(f"s5_hT{i}", [128, 16, 512], BF16) for i in range(2)]
        psu = [ps(f"s5_psu{i}", [128, 512]) for i in range(2)]
        psb = [ps(f"s5_psb{i}", [128, 512]) for i in range(2)]
        psy = ps("s5_psy", [128, 512])
        u32 = sb("a1_u32", [128, 2, 512]); ub = sb("a1_ub", [128, 2, 512], BF16)
        t1 = sb("a1_t1", [128, 256]); t2 = sb("a1_t2", [128, 256])
        vre = sb("a1_vre", [128, 256]); vim = sb("a1_vim", [128, 256])
        wre = [sb(f"a1_wre{i}", [128, 256]) for i in range(2)]; wim = [sb(f"a1_wim{i}", [128, 256]) for i in range(2)]
        p1 = sb("a1_p1", [128, 256]); p2 = sb("a1_p2", [128, 256])
        sreb = [sb(f"a1_sreb{i}", [128, 256], BF16) for i in range(2)]; simb = [sb(f"a1_simb{i}", [128, 256], BF16) for i in range(2)]
        yv = sb("a1_y", [128, 512]); y2 = sb("a1_y2", [128, 512]); sg = sb("a1_sg", [128, 512])
        zb = [sb(f"a1_zb{i}", [128, 512], BF16) for i in range(2)]
        for ci in range(K.opts.get("nch", 16)):
            hs = ci % 2
            sc.op("sp", lambda e, ci=ci, hs=hs: e.dma_start(out=hT[hs][:].rearrange("p k t -> p (k t)"), in_=K.hT_d[ci]),
                  reads=[("hT_d", ci)], writes=[f"hT{hs}"], chan=f"s5_hl{hs}")
            for m in range(2):
                for k in range(16):
                    sc.op("pe", lambda e, m=m, k=k, hs=hs: e.matmul(psu[m][:], lhsT=wu[:, k, m * 128:(m + 1) * 128], rhs=hT[hs][:, k, :],
                                                                    start=(k == 0), stop=(k == 15)),
                          reads=["s5_wu", f"hT{hs}"], writes=[f"psu{m}"], pe_chain=True)
                sc.op("act", lambda e, m=m: e.copy(out=u32[:, m, :], in_=psu[m][:]), reads=[f"psu{m}"], writes=[f"u32_{m}"])
                if STOP <= 1:
                    continue
                sc.op("dve", lambda e, m=m: e.tensor_copy(out=ub[:, m, :], in_=psu[m][:]), reads=[f"psu{m}"], writes=[f"ub_{m}"])
            if STOP <= 1:
                continue
            for m in range(2):
              for pr in range(2):
                for half in range(2):
                    cols = slice(half * 256, half * 256 + 256)
                    for s2 in range(2):
                        s4 = pr * 2 + s2
                        k4 = m * 4 + s4
                        b = s2
                        sc.op("pe", lambda e, k4=k4, m=m, cols=cols: e.matmul(psb[0][:, 0:256], lhsT=BT[:, k4 * 2, :], rhs=ub[:, m, cols], start=True, stop=True),
                              reads=["s5_BT", f"ub_{m}"], writes=["psb0"])
                        sc.op("pe", lambda e, k4=k4, m=m, cols=cols: e.matmul(psb[1][:, 0:256], lhsT=BT[:, k4 * 2 + 1, :], rhs=ub[:, m, cols], start=True, stop=True),
                              reads=["s5_BT", f"ub_{m}"], writes=["psb1"])
                        if STOP <= 2:
                            continue
                        sc.op("dve", lambda e, k4=k4: e.tensor_tensor(out=t1[:], in0=psb[0][:, 0:256], in1=Ct[:, k4, :], op=ALU.mult), reads=["psb0", "s5_tabc"], writes=["t1"])
                        sc.op("dve", lambda e, k4=k4: e.tensor_tensor(out=t2[:], in0=psb[1][:, 0:256], in1=St[:, k4, :], op=ALU.mult), reads=["psb1", "s5_tabs"], writes=["t2"])
                        sc.op("dve", lambda e: e.tensor_tensor(out=vre[:], in0=t1[:], in1=t2[:], op=ALU.add), reads=["t1", "t2"], writes=["vre"])
                        sc.op("dve", lambda e, k4=k4: e.tensor_tensor(out=t1[:], in0=psb[1][:, 0:256], in1=Ct[:, k4, :], op=ALU.mult), reads=["psb1", "s5_tabc"], writes=["t1"])
                        sc.op("dve", lambda e, k4=k4: e.tensor_tensor(out=t2[:], in0=psb[0][:, 0:256], in1=St[:, k4, :], op=ALU.mult), reads=["psb0", "s5_tabs"], writes=["t2"])
                        sc.op("dve", lambda e: e.tensor_tensor(out=vim[:], in0=t1[:], in1=t2[:], op=ALU.subtract), reads=["t1", "t2"], writes=["vim"])
                        if STOP <= 3:
                            continue
                        sc.op("dve", lambda e, k4=k4, b=b: e.tensor_tensor_scan(out=wre[b][:], data0=Amag[:, k4, :], data1=vre[:], initial=ire[:, k4:k4 + 1],
                                                                                 op0=ALU.mult, op1=ALU.add),
                              reads=["s5_Amag", "vre", "s5_ire"], writes=[f"wre{b}"])
                        sc.op("dve", lambda e, k4=k4, b=b: e.tensor_tensor_scan(out=wim[b][:], data0=Amag[:, k4, :], data1=vim[:], initial=iim[:, k4:k4 + 1],
                                                                                 op0=ALU.mult, op1=ALU.add),
                              reads=["s5_Amag", "vim", "s5_iim"], writes=[f"wim{b}"])
                        if STOP <= 4:
                            continue
                        sc.op("dve", lambda e, k4=k4, b=b: e.tensor_scalar(out=itmp[:, k4:k4 + 1], in0=wre[b][:, 255:256], scalar1=c256[:, k4:k4 + 1], scalar2=None, op0=ALU.mult),
                              reads=[f"wre{b}", "s5_r256c"], writes=["s5_itmp"])
                        sc.op("dve", lambda e, k4=k4, b=b: e.scalar_tensor_tensor(out=ire[:, k4:k4 + 1], in0=wim[b][:, 255:256], scalar=ns256[:, k4:k4 + 1], in1=itmp[:, k4:k4 + 1],
                                                                                   op0=ALU.mult, op1=ALU.add),
                              reads=[f"wim{b}", "s5_ns256", "s5_itmp"], writes=["s5_ire"])
                        sc.op("dve", lambda e, k4=k4, b=b: e.tensor_scalar(out=itmp[:, k4:k4 + 1], in0=wre[b][:, 255:256], scalar1=s256[:, k4:k4 + 1], scalar2=None, op0=ALU.mult),
                              reads=[f"wre{b}", "s5_r256s"], writes=["s5_itmp"])
                        sc.op("dve", lambda e, k4=k4, b=b: e.scalar_tensor_tensor(out=iim[:, k4:k4 + 1], in0=wim[b][:, 255:256], scalar=c256[:, k4:k4 + 1], in1=itmp[:, k4:k4 + 1],
                                                                                   op0=ALU.mult, op1=ALU.add),
                              reads=[f"wim{b}", "s5_r256c", "s5_itmp"], writes=["s5_iim"])
                        if STOP <= 5:
                            continue
                        sc.op(PENG, lambda e, k4=k4, b=b: e.tensor_tensor(out=p1[:], in0=wre[b][:], in1=Ct[:, k4, :], op=ALU.mult), reads=[f"wre{b}", "s5_tabc"], writes=["p1"])
                        sc.op(PENG, lambda e, k4=k4, b=b: e.tensor_tensor(out=p2[:], in0=wim[b][:], in1=St[:, k4, :], op=ALU.mult), reads=[f"wim{b}", "s5_tabs"], writes=["p2"])
                        sc.op(PENG, lambda e, b=b: e.tensor_tensor(out=sreb[b][:], in0=p1[:], in1=p2[:], op=ALU.subtract), reads=["p1", "p2"], writes=[f"sreb{b}"])
                        sc.op(PENG, lambda e, k4=k4, b=b: e.tensor_tensor(out=p1[:], in0=wre[b][:], in1=St[:, k4, :], op=ALU.mult), reads=[f"wre{b}", "s5_tabs"], writes=["p1"])
                        sc.op(PENG, lambda e, k4=k4, b=b: e.tensor_tensor(out=p2[:], in0=wim[b][:], in1=Ct[:, k4, :], op=ALU.mult), reads=[f"wim{b}", "s5_tabc"], writes=["p2"])
                        sc.op(PENG, lambda e, b=b: e.tensor_tensor(out=simb[b][:], in0=p1[:], in1=p2[:], op=ALU.add), reads=["p1", "p2"], writes=[f"simb{b}"])
                    for s2 in range(2):
                        if STOP <= 6:
                            continue
                        s4 = pr * 2 + s2
                        k4 = m * 4 + s4
                        b = s2
                        sc.op("pe", lambda e, k4=k4, pr=pr, cols=cols, b=b, s2=s2: e.matmul(psy[pr * 64:pr * 64 + 64, cols], lhsT=CTb[:, k4, :], rhs=sreb[b][:], start=(s2 == 0), stop=False),
                              reads=["s5_CTb", f"sreb{b}"], writes=["psy"], pe_chain=True)
                        sc.op("pe", lambda e, k4=k4, pr=pr, cols=cols, b=b, s2=s2: e.matmul(psy[pr * 64:pr * 64 + 64, cols], lhsT=nCTb[:, k4, :], rhs=simb[b][:], start=False, stop=(s2 == 1)),
                              reads=["s5_nCTb", f"simb{b}"], writes=["psy"], pe_chain=True)
              if STOP <= 7:
                  continue
              zs = (ci * 2 + m) % 2
              sc.op("dve", lambda e, m=m: e.scalar_tensor_tensor(out=yv[:], in0=u32[:, m, :], scalar=d_t[:, m:m + 1], in1=psy[:], op0=ALU.mult, op1=ALU.add),
                    reads=[f"u32_{m}", "s5_d_t", "psy"], writes=["yv"])
              sc.op(PENG, lambda e: e.tensor_tensor(out=y2[:], in0=yv[:], in1=yv[:], op=ALU.mult), reads=["yv"], writes=["y2"])
              sc.op(PENG, lambda e: e.tensor_scalar(out=y2[:], in0=y2[:], scalar1=0.044715, scalar2=1.0, op0=ALU.mult, op1=ALU.add), reads=["y2"], writes=["y2"])
              sc.op(PENG, lambda e: e.tensor_tensor(out=y2[:], in0=y2[:], in1=yv[:], op=ALU.mult), reads=["y2", "yv"], writes=["y2"])
              sc.op("act", lambda e: e.activation(out=sg[:], in_=y2[:], func=AF.Sigmoid, scale=1.5957691216057308), reads=["y2"], writes=["sg"])
              sc.op("dve", lambda e, zs=zs: e.tensor_tensor(out=zb[zs][:], in0=sg[:], in1=yv[:], op=ALU.mult), reads=["sg", "yv"], writes=[f"zb{zs}"])
              sc.op("sp", lambda e, m=m, ci=ci, zs=zs: e.dma_start(out=K.z_d[hq * 256 + m * 128:hq * 256 + (m + 1) * 128, ci * 512:(ci + 1) * 512], in_=zb[zs][:]),
                    reads=[f"zb{zs}"], writes=[("z_d", hq, m, ci)], chan=f"s5_zst{zs}")
        K.sc.op("sp", lambda e: e.dma_start(out=K.z_d[0:1, 0:2], in_=K.z_d[0:1, 0:2]) if False else e.dma_start(out=zb[0][0:1, 0:2], in_=K.z_d[hq * 256:hq * 256 + 1, 0:2]),
                reads=[("z_d", hq, m, ci) for m in range(2) for ci in range(K.opts.get("nch", 16))], writes=["zb0", ("z_done", hq)], chan="s5_zdone")


def gelu_tanh(K, eng2, y, tmp, sg, out, n, keys):
    sc = K.sc
    ky, kt, ks, ko = keys
    sc.op(eng2, lambda e: e.tensor_tensor(out=tmp, in0=y, in1=y, op=ALU.mult), reads=[ky], writes=[kt])
    sc.op(eng2, lambda e: e.tensor_scalar(out=tmp, in0=tmp, scalar1=0.044715, scalar2=1.0, op0=ALU.mult, op1=ALU.add), reads=[kt], writes=[kt])
    sc.op(eng2, lambda e: e.tensor_tensor(out=tmp, in0=tmp, in1=y, op=ALU.mult), reads=[kt, ky], writes=[kt])
    sc.op("act", lambda e: e.activation(out=sg, in_=tmp, func=AF.Sigmoid, scale=1.5957691216057308), reads=[kt], writes=[ks])
    sc.op("dve", lambda e: e.tensor_tensor(out=out, in0=sg, in1=y, op=ALU.mult), reads=[ks, ky], writes=[ko])


def pass_att(K, hq):
    nc, sc, I = K.nc, K.sc, K.I
    with contextlib.ExitStack() as sth:
        sbh = lambda n, s, d=F32: K.sb(n, s, d, sth)
        KsT = sbh("KsT", [128, S], BF16); KwT = sbh("KwT", [128, S], BF16)
        qT = sbh("qT", [128, 2, 2560], BF16)
        vs_tok = sbh("vs_tok", [128, 64, 65], BF16); vw_tok = sbh("vw_tok", [128, 64, 65], BF16)
        gat = sbh("gat", [128, 20, 12])
        KcT = sbh("KcT", [128, 512], BF16); vc_tok = sbh("vc_tok", [128, 4, 65], BF16)
        sc.op("pool", lambda e: e.memset(vs_tok[:], 1.0), writes=["vs_tok"])
        sc.op("pool", lambda e: e.memset(vw_tok[:], 1.0), writes=["vw_tok"])
        sc.op("pool", lambda e: e.memset(vc_tok[:], 1.0), writes=["vc_tok"])
        sc.op("pool", lambda e: e.memset(KcT[:], 0.0), writes=["KcT"])
        with contextlib.ExitStack() as st:
            sb = lambda n, s, d=F32: K.sb(n, s, d, st)
            ps = lambda n, s, d=F32: K.ps(n, s, d, st)
            kcvcT = sb("kcvcT", [128, S], BF16)
            wfm = sb("wfm", [128, 16, 640], BF16); wtm = sb("wtm", [128, 16, 140], BF16)
            Pm = sb("Pm", [128, 128], BF16)
            sc.op("pool", lambda e: e.dma_start(out=wfm[:], in_=I["w_fm"][hq].rearrange("(k p) n -> p k n", p=128)), writes=["wfm"], chan="a2_wfm")
            sc.op("pool", lambda e: e.dma_start(out=wtm[:], in_=I["w_tm"][hq].rearrange("(k p) n -> p k n", p=128)), writes=["wtm"], chan="a2_wtm")
            sc.op("sp", lambda e: e.dma_start(out=Pm[:], in_=I["Pm"]), writes=["Pm"], chan="a2_Pm")
            hT = [sb(f"a2_hT{i}", [128, 16, 512], BF16) for i in range(2)]
            rt = [sb(f"a2_rt{i}", [128, 4, 512]) for i in range(2)]
            xb = sb("a2_xb", [128, 512], BF16); t1 = sb("a2_t1", [128, 512]); t2 = sb("a2_t2", [128, 512])
            psf = [ps(f"a2_psf{i}", [128, 512]) for i in range(2)]
            psr = ps("a2_psr", [128, 512])
            pst = [ps(f"a2_pst{i}", [128, 512]) for i in range(2)]
            nf = 0
            for ci in range(16):
                hs = ci % 2
                cs = slice(ci * 512, (ci + 1) * 512)
                sc.op("sp", lambda e, ci=ci, hs=hs: e.dma_start(out=hT[hs][:].rearrange("p k t -> p (k t)"), in_=K.hT_d[ci]),
                      reads=[("hT_d", ci)], writes=[f"a2_hT{hs}"], chan=f"a2_hl{hs}")
                sc.op("sp", lambda e, hs=hs, cs=cs: [e.dma_start(out=rt[hs][:, j_, :], in_=I[nm][:, cs])
                                                     for j_, nm in enumerate(("ropeC", "ropeS", "ropeCk", "ropeSk"))],
                      writes=[f"a2_rt{hs}"], chan=f"a2_rt{hs}")
                tiles = [(2, KsT[:, cs], 0), (3, KwT[:, cs], 0), (4, kcvcT[:, cs], 2)]
                if ci >= 11:
                    qs = slice((ci - 11) * 512, (ci - 10) * 512)
                    tiles += [(0, qT[:, 0, qs], 0), (1, qT[:, 1, qs], 0)]
                for (mt, dst, tb) in tiles:
                    pb = nf % 2
                    nf += 1
                    for k in range(16):
                        sc.op("pe", lambda e, mt=mt, k=k, hs=hs, pb=pb: e.matmul(psf[pb][:], lhsT=wfm[:, k, mt * 128:(mt + 1) * 128], rhs=hT[hs][:, k, :],
                                                                               start=(k == 0), stop=(k == 15)),
                              reads=["wfm", f"a2_hT{hs}"], writes=[f"a2_psf{pb}"], pe_chain=True)
                    sc.op("act", lambda e, pb=pb: e.copy(out=xb[:], in_=psf[pb][:]), reads=[f"a2_psf{pb}"], writes=["a2_xb"])
                    sc.op("pe", lambda e: e.matmul(psr[:], lhsT=Pm[:], rhs=xb[:], start=True, stop=True), reads=["Pm", "a2_xb"], writes=["a2_psr"])
                    sc.op("dve", lambda e, pb=pb, hs=hs, tb=tb: e.tensor_tensor(out=t1[:], in0=psf[pb][:], in1=rt[hs][:, tb, :], op=ALU.mult),
                          reads=[f"a2_psf{pb}", f"a2_rt{hs}"], writes=["a2_t1"])
                    sc.op("dve", lambda e, hs=hs, tb=tb: e.tensor_tensor(out=t2[:], in0=psr[:], in1=rt[hs][:, tb + 1, :], op=ALU.mult),
                          reads=["a2_psr", f"a2_rt{hs}"], writes=["a2_t2"])
                    sc.op("pool", lambda e, dst=dst: e.tensor_tensor(out=dst, in0=t1[:], in1=t2[:], op=ALU.add),
                          reads=["a2_t1", "a2_t2"], writes=[("fm", mt)])
                for tt in range(4):
                    ti = ci * 4 + tt
                    pb = ti % 2
                    for k in range(16):
                        sc.op("pe", lambda e, k=k, hs=hs, tt=tt, pb=pb: e.matmul(pst[pb][:, 0:140], lhsT=hT[hs][:, k, tt * 128:(tt + 1) * 128], rhs=wtm[:, k, :],
                                                                               start=(k == 0), stop=(k == 15)),
                              reads=["wtm", f"a2_hT{hs}"], writes=[f"a2_pst{pb}"], pe_chain=True)
                    sc.op("act", lambda e, ti=ti, pb=pb: e.copy(out=vs_tok[:, ti, 0:64], in_=pst[pb][:, 0:64]), reads=[f"a2_pst{pb}"], writes=["vs_tok"])
                    sc.op("dve", lambda e, ti=ti, pb=pb: e.tensor_copy(out=vw_tok[:, ti, 0:64], in_=pst[pb][:, 64:128]), reads=[f"a2_pst{pb}"], writes=["vw_tok"])
                    if ti >= QT0:
                        sc.op("act", lambda e, ti=ti, pb=pb: e.activation(out=gat[:, ti - QT0, :], in_=pst[pb][:, 128:140], func=AF.Sigmoid),
                              reads=[f"a2_pst{pb}"], writes=["gat"])
            if K.opts.get("att_stop", 99) <= 1:
                return
            W1 = sb("W1", [128, 32, 128], BF16)
            sc.op("pool", lambda e: e.dma_start(out=W1[0:64], in_=I["w1_k"].rearrange("(l d) j -> d l j", d=64)), writes=["W1k"], chan="c_W1k")
            sc.op("pool", lambda e: e.dma_start(out=W1[64:128], in_=I["w1_v"].rearrange("(l d) j -> d l j", d=64)), writes=["W1v"], chan="c_W1v")
            peT = sb("peT", [128, 32], BF16)
            sc.op("pool", lambda e: e.dma_start(out=peT[0:64], in_=I["pe_kT"]), writes=["peTk"], chan="c_pek")
            sc.op("pool", lambda e: e.dma_start(out=peT[64:128], in_=I["pe_vT"]), writes=["peTv"], chan="c_pev")
            W2k = sb("W2k", [128, 128], BF16); W2v = sb("W2v", [128, 64], BF16)
            sc.op("pool", lambda e: e.dma_start(out=W2k[:, 0:64], in_=I["w2_k"]), writes=["W2ka"], chan="c_w2a")
            sc.op("pool", lambda e: e.dma_start(out=W2k[:, 64:128], in_=I["w2_k"]), writes=["W2kb"], chan="c_w2b")
            sc.op("pool", lambda e: e.dma_start(out=W2v[:], in_=I["w2_v"]), writes=["W2v"], chan="c_w2v")
            biasS = sb("c_bias", [128, 2]); yh = sb("c_yh", [128, 512]); th = sb("c_th", [128, 512]); sgh = sb("c_sg", [128, 512])
            hb = [sb(f"c_hb{i}", [128, 512], BF16) for i in range(2)]
            psh = psf[0]; psbias = psr; pso = psf[1]
            kv = kcvcT[:].rearrange("p (n s) -> p n s", s=16)
            for si in range(2):
                rows = slice(si * 64, si * 64 + 64)
                for l in range(32):
                    sc.op("pe", lambda e, si=si, rows=rows, l=l: e.matmul(psbias[:, si:si + 1], lhsT=W1[rows, l, :], rhs=peT[rows, l:l + 1], start=(l == 0), stop=(l == 31)),
                          reads=["W1k", "W1v", "peTk", "peTv"], writes=["a2_psr"], pe_chain=True)
                sc.op("act", lambda e, si=si: e.copy(out=biasS[:, si:si + 1], in_=psbias[:, si:si + 1]), reads=["a2_psr"], writes=["c_bias"])
                for l in range(32):
                    rhs = kv[rows, 0:511, l] if l < 16 else kv[rows, 1:512, l - 16]
                    sc.op("pe", lambda e, rows=rows, l=l, rhs=rhs: e.matmul(psh[:, 0:511], lhsT=W1[rows, l, :], rhs=rhs, start=(l == 0), stop=(l == 31)),
                          reads=["W1k", "W1v", ("fm", 4)], writes=["a2_psf0"], pe_chain=True)
                sc.op("act", lambda e, si=si: e.activation(out=yh[:, 0:511], in_=psh[:, 0:511], func=AF.Identity, bias=biasS[:, si:si + 1]),
                      reads=["a2_psf0", "c_bias"], writes=["c_yh"])
                sc.op("pool", lambda e, si=si: e.memset(hb[si][:], 0.0), writes=[f"c_hb{si}"])
                gelu_tanh(K, "pool", yh[:, 0:511], th[:, 0:511], sgh[:, 0:511], hb[si][:, 0:511], 511, ("c_yh", "c_th", "c_sg", f"c_hb{si}"))
            sc.op("pe", lambda e: e.matmul(pso[:, 0:511], lhsT=W2k[:], rhs=hb[0][:, 0:511], start=True, stop=True), reads=["W2ka", "W2kb", "c_hb0"], writes=["a2_psf1"])
            sc.op("act", lambda e: e.copy(out=KcT[:, 0:511], in_=pso[:, 0:511]), reads=["a2_psf1"], writes=["KcT"])
            for c in range(4):
                sc.op("pe", lambda e, c=c: e.matmul(psr[:, 0:64], lhsT=hb[1][:, c * 128:(c + 1) * 128], rhs=W2v[:], start=True, stop=True),
                      reads=["W2v", "c_hb1"], writes=["a2_psr"])
                sc.op("act", lambda e, c=c: e.copy(out=vc_tok[:, c, 0:64], in_=psr[:, 0:64]), reads=["a2_psr"], writes=["vc_tok"])
            if "kc" in K.dbg and hq == K.opts.get("dbg_hq", 0):
                dbg_dump(K, "kc", KcT[:], [128, 512], BF16, ["KcT"])
                dbg_dump(K, "ks", KsT[:], [128, S], BF16, [("fm", 2)])
                dbg_dump(K, "q", qT[:], [128, 2, 2560], BF16, [("fm", 0), ("fm", 1)])
        if K.opts.get("att_stop", 99) <= 2:
            return
        sc.barrier()
        with contextlib.ExitStack() as st:
            sb = lambda n, s, d=F32: K.sb(n, s, d, st)
            ps = lambda n, s, d=F32: K.ps(n, s, d, st)
            Zexp = sb("Zexp", [128, S], BF16); Wimp = sb("Wimp", [128, 4, 128], BF16); causal4 = sb("causal4", [128, 512], BF16)
            bv = sb("bv", [128, 128])
            sc.op("sp", lambda e: e.dma_start(out=Zexp[:], in_=I["Zexp"]), writes=["Zexp"], chan="b_Z")
            sc.op("sp", lambda e: e.dma_start(out=Wimp[:], in_=I["Wimp"]), writes=["Wimp"], chan="b_W")
            sc.op("sp", lambda e: e.dma_start(out=causal4[:], in_=I["causal4"]), writes=["causal4"], chan="b_c4")
            sc.op("sp", lambda e: e.dma_start(out=bv[:], in_=I["blockvalid"]), writes=["bv"], chan="b_bv")
            cmpm = sb("cmpm", [128, 4, 512], BF16); winm = sb("winm", [128, 5, 512], BF16); selb = sb("selb", [128, 128])
            pss = [ps(f"b_pss{i}", [128, 512]) for i in range(2)]
            psm = ps("b_psm", [128, 512]); oacc = ps("b_oacc", [128, 512]); psimp = ps("b_psimp", [128, 512])
            pTA = ps("b_pTA", [128, 512]); pTB = ps("b_pTB", [128, 512]); pmb = ps("b_pmb", [128, 1024], BF16)
            ec = [sb(f"b_ec{i}", [128, 512], BF16) for i in range(4)]
            ee = [sb(f"b_e{i}", [128, 512], BF16) for i in range(2)]
            oS = sb("b_oS", [65, 3, 512])
            den = sb("b_den", [128, 4]); impa = sb("b_impa", [128, 128]); impb = sb("b_impb", [128, 128])
            m1 = sb("b_m1", [128, 8]); m2 = sb("b_m2", [128, 8]); selm = sb("b_selm", [128, 128], BF16); selT4 = sb("b_selT4", [128, 512], BF16)
            dens = sb("b_dens", [128, 12]); coef = sb("b_coef", [128, 12]); yatt = sb("b_yatt", [128, 256]); yb = sb("b_yb", [128, 256], BF16)
            yT = sb("b_yT", [128, 2, 128], BF16)
            qn = [0]

            def scores(KT, kt, qi, pb):
                for g in (0, 2, 1, 3):
                    r = slice((g % 2) * 64, (g % 2) * 64 + 64)
                    sc.op("pe", lambda e, g=g, r=r, kt=kt, qi=qi, pb=pb: e.matmul(pss[pb][:, g * 128:(g + 1) * 128], lhsT=KT[r, kt * 128:(kt + 1) * 128],
                                                                                   rhs=qT[r, g // 2, qi * 128:(qi + 1) * 128], start=True, stop=True),
                          reads=[("fm", 0), ("fm", 1), ("fm", 2), ("fm", 3), "KcT"], writes=[f"b_pss{pb}"], pe_chain=(g != 1))

            for qj in range(K.opts.get("nq", NQ)):
                qt = QF + qj
                qi = qt - QT0
                sc.op("sp", lambda e, qj=qj: e.dma_start(out=cmpm[:], in_=I["cmpmask"][qj]), writes=["cmpm"], chan="b_cmpm")
                sc.op("sp", lambda e, qj=qj: e.dma_start(out=winm[:], in_=I["winmask"][qj]), writes=["winm"], chan="b_winm")
                sc.op("sp", lambda e, qj=qj: e.dma_start(out=selb[:], in_=I["selbias"][qj]), writes=["selb"], chan="b_selb")
                for c in range(4):
                    pb = qn[0] % 2; qn[0] += 1
                    scores(KcT, c, qi, pb)
                    sc.op("act", lambda e, c=c, pb=pb: e.activation(out=ec[c][:], in_=pss[pb][:], func=AF.Exp, scale=0.125), reads=[f"b_pss{pb}"], writes=[f"b_ec{c}"])
                    sc.op("dve", lambda e, c=c: e.tensor_tensor(out=ec[c][:], in0=ec[c][:], in1=cmpm[:, c, :], op=ALU.mult), reads=[f"b_ec{c}", "cmpm"], writes=[f"b_ec{c}"])
                    sc.op("pe", lambda e, c=c: e.matmul(oacc[0:65, :], lhsT=vc_tok[:, c, :], rhs=ec[c][:], start=(c == 0), stop=(c == 3)),
                          reads=["vc_tok", f"b_ec{c}"], writes=["b_oacc"], pe_chain=True)
                sc.op("act", lambda e: e.copy(out=oS[:, 0, :], in_=oacc[0:65, :]), reads=["b_oacc"], writes=["b_oS0"])
                for g in range(4):
                    for c in range(4):
                        sc.op("pe", lambda e, g=g, c=c: e.matmul(psimp[:, g * 128:(g + 1) * 128], lhsT=ec[c][:, g * 128:(g + 1) * 128], rhs=Wimp[:, c, :],
                                                               start=(c == 0), stop=(c == 3)),
                              reads=[f"b_ec{c}", "Wimp"], writes=["b_psimp"], pe_chain=True)
                for g in range(4):
                    for c in range(4):
                        sc.op("pe", lambda e, g=g, c=c: e.matmul(pTA[:, 400 + g:401 + g], lhsT=ec[c][:, g * 128:(g + 1) * 128], rhs=K.ones_col[:],
                                                               start=(c == 0), stop=(c == 3)),
                              reads=[f"b_ec{c}", "ones_col"], writes=["b_pTA"], pe_chain=True)
                sc.op("dve", lambda e: e.tensor_scalar(out=den[:], in0=pTA[:, 400:404], scalar1=1e-30, scalar2=None, op0=ALU.add), reads=["b_pTA"], writes=["b_den"])
                sc.op("dve", lambda e: e.reciprocal(out=den[:], in_=den[:]), reads=["b_den"], writes=["b_den"])
                for g in range(4):
                    sc.op("dve", lambda e, g=g: e.scalar_tensor_tensor(out=impa[:], in0=psimp[:, g * 128:(g + 1) * 128], scalar=den[:, g:g + 1],
                                                                       in1=(selb[:] if g == 0 else impa[:]), op0=ALU.mult, op1=ALU.add),
                          reads=["b_psimp", "b_den", "selb", "b_impa"], writes=["b_impa"])
                sc.op("dve", lambda e: e.max(out=m1[:], in_=impa[:]), reads=["b_impa"], writes=["b_m1"])
                sc.op("dve", lambda e: e.match_replace(out=impb[:], in_to_replace=m1[:], in_values=impa[:], imm_value=-3.0e38), reads=["b_m1", "b_impa"], writes=["b_impb"])
                sc.op("dve", lambda e: e.max(out=m2[:], in_=impb[:]), reads=["b_impb"], writes=["b_m2"])
                sc.op("dve", lambda e: e.scalar_tensor_tensor(out=selm[:], in0=impa[:], scalar=m2[:, 7:8], in1=bv[:], op0=ALU.is_ge, op1=ALU.mult),
                      reads=["b_impa", "b_m2", "bv"], writes=["b_selm"])
                sc.op("pe", lambda e: e.transpose(out=pmb[:, 0:128], in_=selm[:], identity=K.ident_bf[:]), reads=["b_selm", "ident_bf"], writes=["b_pmb"])
                for r4 in range(4):
                    sc.op("act" if r4 % 2 == 0 else "dve",
                          (lambda e, r4=r4: e.copy(out=selT4[:, r4 * 128:(r4 + 1) * 128], in_=pmb[:, 0:128])) if r4 % 2 == 0 else
                          (lambda e, r4=r4: e.tensor_copy(out=selT4[:, r4 * 128:(r4 + 1) * 128], in_=pmb[:, 0:128])),
                          reads=["b_pmb"], writes=["b_selT4"])
                for kt in range(qt + 1):
                    pb = qn[0] % 2; qn[0] += 1
                    eb = kt % 2
                    scores(KsT, kt, qi, pb)
                    sc.op("act", lambda e, eb=eb, pb=pb: e.activation(out=ee[eb][:], in_=pss[pb][:], func=AF.Exp, scale=0.125), reads=[f"b_pss{pb}"], writes=[f"b_e{eb}"])
                    if kt == qt:
                        sc.op("dve", lambda e, eb=eb: e.tensor_tensor(out=ee[eb][:], in0=ee[eb][:], in1=causal4[:], op=ALU.mult), reads=[f"b_e{eb}", "causal4"], writes=[f"b_e{eb}"])
                    else:
                        sc.op("pe", lambda e, kt=kt: e.matmul(psm[:], lhsT=Zexp[:, kt * 128:(kt + 1) * 128], rhs=selT4[:], start=True, stop=True),
                              reads=["Zexp", "b_selT4"], writes=["b_psm"])
                        sc.op("dve", lambda e, eb=eb: e.tensor_tensor(out=ee[eb][:], in0=ee[eb][:], in1=psm[:], op=ALU.mult), reads=[f"b_e{eb}", "b_psm"], writes=[f"b_e{eb}"])
                    sc.op("pe", lambda e, kt=kt, eb=eb, qt=qt: e.matmul(oacc[0:65, :], lhsT=vs_tok[:, kt, :], rhs=ee[eb][:], start=(kt == 0), stop=(kt == qt)),
                          reads=["vs_tok", f"b_e{eb}"], writes=["b_oacc"], pe_chain=True)
                sc.op("act", lambda e: e.copy(out=oS[:, 1, :], in_=oacc[0:65, :]), reads=["b_oacc"], writes=["b_oS1"])
                for w in range(5):
                    kt = qt - 4 + w
                    pb = qn[0] % 2; qn[0] += 1
                    eb = w % 2
                    scores(KwT, kt, qi, pb)
                    sc.op("act", lambda e, eb=eb, pb=pb: e.activation(out=ee[eb][:], in_=pss[pb][:], func=AF.Exp, scale=0.125), reads=[f"b_pss{pb}"], writes=[f"b_e{eb}"])
                    sc.op("dve", lambda e, eb=eb, w=w: e.tensor_tensor(out=ee[eb][:], in0=ee[eb][:], in1=winm[:, w, :], op=ALU.mult), reads=[f"b_e{eb}", "winm"], writes=[f"b_e{eb}"])
                    sc.op("pe", lambda e, kt=kt, eb=eb, w=w: e.matmul(oacc[0:65, :], lhsT=vw_tok[:, kt, :], rhs=ee[eb][:], start=(w == 0), stop=(w == 4)),
                          reads=["vw_tok", f"b_e{eb}"], writes=["b_oacc"], pe_chain=True)
                sc.op("act", lambda e: e.copy(out=oS[:, 2, :], in_=oacc[0:65, :]), reads=["b_oacc"], writes=["b_oS2"])
                for br in range(3):
                    for g in range(4):
                        t = br * 4 + g
                        pT = pTA if t < 6 else pTB
                        sc.op("pe", lambda e, br=br, g=g, t=t, pT=pT: e.transpose(out=pT[:, (t % 6) * 65:(t % 6) * 65 + 65], in_=oS[:, br, g * 128:(g + 1) * 128],
                                                                                 identity=K.ident_f[0:65, 0:65]),
                              reads=[f"b_oS{br}", "ident_f"], writes=["b_pTA" if t < 6 else "b_pTB"], pe_chain=True)
                sc.op("dve", lambda e: e.tensor_copy(out=dens[:, 0:6], in_=pTA[:, 0:390].rearrange("p (t c) -> p t c", c=65)[:, :, 64]), reads=["b_pTA"], writes=["b_dens"])
                sc.op("dve", lambda e: e.tensor_copy(out=dens[:, 6:12], in_=pTB[:, 0:390].rearrange("p (t c) -> p t c", c=65)[:, :, 64]), reads=["b_pTB"], writes=["b_dens"])
                sc.op("dve", lambda e: e.tensor_scalar(out=dens[:], in0=dens[:], scalar1=1e-30, scalar2=None, op0=ALU.add), reads=["b_dens"], writes=["b_dens"])
                sc.op("dve", lambda e: e.reciprocal(out=dens[:], in_=dens[:]), reads=["b_dens"], writes=["b_dens"])
                sc.op("dve", lambda e, qi=qi: e.tensor_tensor(out=coef[:], in0=dens[:], in1=gat[:, qi, :], op=ALU.mult), reads=["b_dens", "gat"], writes=["b_coef"])
                for g in range(4):
                    for br in range(3):
                        t = br * 4 + g
                        pT = pTA if t < 6 else pTB
                        src = pT[:, (t % 6) * 65:(t % 6) * 65 + 64]
                        if br == 0:
                            sc.op("dve", lambda e, g=g, t=t, src=src: e.tensor_scalar(out=yatt[:, g * 64:(g + 1) * 64], in0=src, scalar1=coef[:, t:t + 1], scalar2=None, op0=ALU.mult),
                                  reads=["b_pTA", "b_pTB", "b_coef"], writes=["b_yatt"])
                        else:
                            sc.op("dve", lambda e, g=g, t=t, src=src: e.scalar_tensor_tensor(out=yatt[:, g * 64:(g + 1) * 64], in0=src, scalar=coef[:, t:t + 1],
                                                                                            in1=yatt[:, g * 64:(g + 1) * 64], op0=ALU.mult, op1=ALU.add),
                                  reads=["b_pTA", "b_pTB", "b_coef", "b_yatt"], writes=["b_yatt"])
                sc.op("act", lambda e: e.copy(out=yb[:], in_=yatt[:]), reads=["b_yatt"], writes=["b_yb"])
                for h2 in range(2):
                    sc.op("pe", lambda e, h2=h2: e.transpose(out=pmb[:, 256 + h2 * 128:384 + h2 * 128], in_=yb[:, h2 * 128:(h2 + 1) * 128], identity=K.ident_bf[:]),
                          reads=["b_yb", "ident_bf"], writes=["b_pmb"], pe_chain=True)
                sc.op("act", lambda e: e.copy(out=yT[:].rearrange("p a b -> p (a b)"), in_=pmb[:, 256:512]), reads=["b_pmb"], writes=["b_yT"])
                for h2 in range(2):
                    r0 = hq * 256 + h2 * 128
                    sc.op("sp", lambda e, h2=h2, r0=r0, qj=qj: e.dma_start(out=K.a_d[r0:r0 + 128, qj * 128:(qj + 1) * 128], in_=yT[:, h2, :]),
                          reads=["b_yT"], writes=[("a_d", hq, qj, h2)], chan=f"b_ast{h2}")
            sc.op("sp", lambda e: e.dma_start(out=yT[0:1, 0, 0:2], in_=K.a_d[hq * 256:hq * 256 + 1, 0:2]),
                  reads=[("a_d", hq, qj, h2) for qj in range(K.opts.get("nq", NQ)) for h2 in range(2)], writes=["b_yT", ("a_done", hq)], chan="b_adone")


def phase2a(K):
    nc, sc, I = K.nc, K.sc, K.I
    heads = K.opts.get("heads", (0, 1, 2, 3))
    zdeps = [("z_done", h) for h in heads]
    adeps = [("a_done", h) for h in heads]
    with contextlib.ExitStack() as st:
        sb = lambda n, s, d=F32: K.sb(n, s, d, st)
        ps = lambda n, s, d=F32: K.ps(n, s, d, st)
        wglu = sb("wglu", [128, 8, 1024], BF16)
        sc.op("pool", lambda e: e.dma_start(out=wglu[:], in_=I["w_glu"].rearrange("(k p) n -> p k n", p=128)), writes=["wglu"], chan="p2_wglu")
        bglu = sb("bglu", [128, 8]); gss = sb("gss", [128, 8]); gns = sb("gns", [128, 8])
        sc.op("sp", lambda e: [e.dma_start(out=bglu[:], in_=I["b_glu_t"]), e.dma_start(out=gss[:], in_=I["g_ssm_t"]), e.dma_start(out=gns[:], in_=I["g_nsa_t"])],
              writes=["p2_vecs"], chan="p2_vecs")
        ga1B = sb("ga1B", [128, D]); scale2B = sb("scale2B", [128, D]); shift2B = sb("shift2B", [128, D]); gfB = sb("gfB2", [128, D])
        bcast_row(K, ga1B, 2, "ga1B"); bcast_row(K, shift2B, 3, "shift2B"); bcast_row(K, scale2B, 4, "scale2B")
        sc.op("sp", lambda e: e.dma_start(out=gfB[:], in_=I["g_ffn"].partition_broadcast(128)), writes=["gfB2"], chan="p2_gfB")
        sc.op("dve", lambda e: e.scalar_tensor_tensor(out=scale2B[:], in0=scale2B[:], scalar=1.0, in1=gfB[:], op0=ALU.add, op1=ALU.mult),
              reads=["scale2B", "gfB2"], writes=["scale2B"])
        wo = [sb(f"wo{i}", [128, 16, 512], BF16) for i in range(2)]
        zT = sb("zT", [128, 8, 256], BF16); aT = sb("aT", [128, 8, 256], BF16)
        ygs = sb("ygs", [128, 8, 256], BF16); yga = sb("yga", [128, 8, 256], BF16); sqs = sb("sqs", [128, 8, 256], BF16); sqa = sb("sqa", [128, 8, 256], BF16)
        sig = sb("sig", [128, 256]); ysf = sb("ysf", [128, 256])
        xm = sb("xm", [128, 2, D]); tmpo = sb("tmpo", [128, 512]); junk = sb("junk2", [128, D], BF16)
        ss = sb("ss2a", [128, 2, 2]); r2 = sb("r2a", [128, 2, 2]); ssn = sb("ssn", [128, 2]); rn = sb("rn", [128, 2])
        h2f = sb("h2f", [128, D]); h2b = sb("h2b", [128, D], BF16)
        h2T = sb("h2T", [128, 16, 256], BF16)
        psg = [ps(f"p2_psg{i}", [128, 512]) for i in range(2)]
        psA = ps("p2_psA", [128, 512]); psB = ps("p2_psB", [128, 512]); psst = ps("p2_psst", [128, 512])
        ptr = [ps(f"p2_ptr{i}", [128, 8, 128], BF16) for i in range(2)]
        wsrc = I["w_out"].rearrange("(k p) n -> p k n", p=128)
        blocks = [(2 + 256 * i, 256) for i in range(8)] + [(0, 2)]
        wn = 0
        for (c0, n) in blocks:
            nt_ = (n + 127) // 128
            sc.op("sp", lambda e, c0=c0, n=n: e.dma_start(out=zT[:, :, 0:n], in_=K.z_d.rearrange("(k p) t -> p k t", p=128)[:, :, ZC0 + c0:ZC0 + c0 + n]),
                  reads=zdeps, writes=["zT"], chan="p2_zT")
            sc.op("sp", lambda e, c0=c0, n=n: e.dma_start(out=aT[:, :, 0:n], in_=K.a_d.rearrange("(k p) t -> p k t", p=128)[:, :, 126 + c0:126 + c0 + n]),
                  reads=adeps, writes=["aT"], chan="p2_aT")
            for m in range(8):
                pb = m % 2
                for k in range(8):
                    sc.op("pe", lambda e, m=m, k=k, pb=pb, n=n: e.matmul(psg[pb][:, 0:n], lhsT=wglu[:, k, m * 128:(m + 1) * 128], rhs=zT[:, k, 0:n], start=(k == 0), stop=(k == 7)),
                          reads=["wglu", "zT"], writes=[f"p2_psg{pb}"], pe_chain=True)
                sc.op("act", lambda e, m=m, pb=pb, n=n: e.activation(out=sig[:, 0:n], in_=psg[pb][:, 0:n], func=AF.Sigmoid, bias=bglu[:, m:m + 1]),
                      reads=[f"p2_psg{pb}", "p2_vecs"], writes=["sig"])
                sc.op("dve", lambda e, m=m, n=n: e.tensor_tensor(out=ysf[:, 0:n], in0=sig[:, 0:n], in1=zT[:, m, 0:n], op=ALU.mult), reads=["sig", "zT"], writes=["ysf"])
                sc.op("dve", lambda e, m=m, n=n: e.tensor_scalar(out=ygs[:, m, 0:n], in0=ysf[:, 0:n], scalar1=gss[:, m:m + 1], scalar2=None, op0=ALU.mult),
                      reads=["ysf", "p2_vecs"], writes=["ygs"])
                sc.op("pool", lambda e, m=m, n=n: e.tensor_tensor(out=sqs[:, m, 0:n], in0=ysf[:, 0:n], in1=ysf[:, 0:n], op=ALU.mult), reads=["ysf"], writes=["sqs"])
                sc.op("pool", lambda e, m=m, n=n: e.tensor_scalar(out=yga[:, m, 0:n], in0=aT[:, m, 0:n], scalar1=gns[:, m:m + 1], scalar2=None, op0=ALU.mult),
                      reads=["aT", "p2_vecs"], writes=["yga"])
                sc.op("pool", lambda e, m=m, n=n: e.tensor_tensor(out=sqa[:, m, 0:n], in0=aT[:, m, 0:n], in1=aT[:, m, 0:n], op=ALU.mult), reads=["aT"], writes=["sqa"])
            for tt in range(nt_):
                M = min(128, n - tt * 128)
                tc_ = slice(tt * 128, tt * 128 + M)
                for j_, sq in enumerate((sqs, sqa)):
                    for k in range(8):
                        sc.op("pe", lambda e, j_=j_, sq=sq, k=k, tt=tt, M=M, tc_=tc_: e.matmul(psst[0:M, tt * 2 + j_:tt * 2 + j_ + 1], lhsT=sq[:, k, tc_], rhs=K.ones_col[:],
                                                                                             start=(k == 0), stop=(k == 7)),
                              reads=["sqs", "sqa", "ones_col"], writes=["p2_psst"], pe_chain=True)
                sc.op("act", lambda e, tt=tt, M=M: e.activation(out=r2[0:M, tt, :], in_=psst[0:M, tt * 2:tt * 2 + 2], func=AF.Ln, scale=1.0 / 1024, bias=EPS),
                      reads=["p2_psst"], writes=["r2a"])
                sc.op("act", lambda e, tt=tt, M=M: e.activation(out=r2[0:M, tt, :], in_=r2[0:M, tt, :], func=AF.Exp, scale=-0.5), reads=["r2a"], writes=["r2a"])
                sc.op("sp", lambda e, tt=tt, M=M, c0=c0: e.dma_start(out=xm[0:M, tt, :], in_=I["x_own"][c0 + tt * 128:c0 + tt * 128 + M, :]), writes=[("xm", tt)], chan=f"p2_xo{tt}")
            for db in range(4):
                ws = wn % 2; wn += 1
                sc.op("pool", lambda e, db=db, ws=ws: e.dma_start(out=wo[ws][:], in_=wsrc[:, :, db * 512:(db + 1) * 512]), writes=[f"wo{ws}"], chan=f"p2_wo{ws}")
                for tt in range(nt_):
                    M = min(128, n - tt * 128)
                    tc_ = slice(tt * 128, tt * 128 + M)
                    for k in range(8):
                        sc.op("pe", lambda e, k=k, ws=ws, M=M, tc_=tc_: e.matmul(psA[0:M, :], lhsT=ygs[:, k, tc_], rhs=wo[ws][:, k, :], start=(k == 0), stop=(k == 7)),
                              reads=["ygs", f"wo{ws}"], writes=["p2_psA"], pe_chain=True)
                    for k in range(8):
                        sc.op("pe", lambda e, k=k, ws=ws, M=M, tc_=tc_: e.matmul(psB[0:M, :], lhsT=yga[:, k, tc_], rhs=wo[ws][:, 8 + k, :], start=(k == 0), stop=(k == 7)),
                              reads=["yga", f"wo{ws}"], writes=["p2_psB"], pe_chain=True)
                    sc.op("act", lambda e, tt=tt, M=M: e.activation(out=tmpo[0:M, :], in_=psA[0:M, :], func=AF.Copy, scale=r2[0:M, tt, 0:1]), reads=["p2_psA", "r2a"], writes=["tmpo"])
                    sc.op("dve", lambda e, tt=tt, M=M: e.scalar_tensor_tensor(out=tmpo[0:M, :], in0=psB[0:M, :], scalar=r2[0:M, tt, 1:2], in1=tmpo[0:M, :], op0=ALU.mult, op1=ALU.add),
                          reads=["p2_psB", "r2a", "tmpo"], writes=["tmpo"])
                    sc.op("dve", lambda e, db=db, M=M: e.tensor_tensor(out=tmpo[0:M, :], in0=tmpo[0:M, :], in1=ga1B[0:M, db * 512:(db + 1) * 512], op=ALU.mult),
                          reads=["tmpo", "ga1B"], writes=["tmpo"])
                    sc.op("dve", lambda e, db=db, tt=tt, M=M: e.tensor_tensor(out=xm[0:M, tt, db * 512:(db + 1) * 512], in0=xm[0:M, tt, db * 512:(db + 1) * 512], in1=tmpo[0:M, :], op=ALU.add),
                          reads=["tmpo", ("xm", tt)], writes=[("xm", tt)])
            for tt in range(nt_):
                M = min(128, n - tt * 128)
                r0 = c0 + tt * 128
                sc.op("sp", lambda e, tt=tt, M=M, r0=r0: e.dma_start(out=K.xm_d[r0:r0 + M, :], in_=xm[0:M, tt, :]), reads=[("xm", tt)], writes=[("xm_d", r0)], chan=f"p2_xms{tt}")
                sc.op("act", lambda e, tt=tt, M=M: e.activation(out=junk[0:M, :], in_=xm[0:M, tt, :], func=AF.Square, accum_out=ssn[0:M, tt:tt + 1]), reads=[("xm", tt)], writes=["junk2", "ssn"])
                sc.op("act", lambda e, tt=tt, M=M: e.activation(out=rn[0:M, tt:tt + 1], in_=ssn[0:M, tt:tt + 1], func=AF.Ln, scale=1.0 / D, bias=EPS), reads=["ssn"], writes=["rn"])
                sc.op("act", lambda e, tt=tt, M=M: e.activation(out=rn[0:M, tt:tt + 1], in_=rn[0:M, tt:tt + 1], func=AF.Exp, scale=-0.5), reads=["rn"], writes=["rn"])
                sc.op("dve", lambda e, tt=tt, M=M: e.scalar_tensor_tensor(out=h2f[0:M, :], in0=xm[0:M, tt, :], scalar=rn[0:M, tt:tt + 1], in1=scale2B[0:M, :], op0=ALU.mult, op1=ALU.mult),
                      reads=[("xm", tt), "rn", "scale2B"], writes=["h2f"])
                sc.op("dve", lambda e, M=M: e.tensor_tensor(out=h2b[0:M, :], in0=h2f[0:M, :], in1=shift2B[0:M, :], op=ALU.add), reads=["h2f", "shift2B"], writes=["h2b"])
                for half in range(2):
                    for k8 in range(8):
                        k = half * 8 + k8
                        sc.op("pe", lambda e, k=k, k8=k8, half=half, M=M: e.transpose(out=ptr[half][:, k8, 0:M], in_=h2b[0:M, k * 128:(k + 1) * 128], identity=K.ident_bf[0:M, 0:M]),
                              reads=["h2b", "ident_bf"], writes=[f"p2_ptr{half}"], pe_chain=True)
                    sc.op("act", lambda e, half=half, tt=tt, M=M: e.copy(out=h2T[:, half * 8:(half + 1) * 8, tt * 128:tt * 128 + M], in_=ptr[half][:, :, 0:M]),
                          reads=[f"p2_ptr{half}"], writes=["h2T"])
            sc.op("sp", lambda e, c0=c0, n=n: e.dma_start(out=K.h2T_d[:, :, c0:c0 + n], in_=h2T[:, :, 0:n]), reads=["h2T"], writes=[("h2T_d", c0)], chan="p2_h2s")
        K.p2_blocks = blocks


def phase2b(K):
    nc, sc, I = K.nc, K.sc, K.I
    hdeps = [("h2T_d", c0) for (c0, n) in K.p2_blocks]
    xdeps = [("xm_d", c0 + tt * 128) for (c0, n) in K.p2_blocks for tt in range((n + 127) // 128)]
    with contextlib.ExitStack() as st0:
        sb0 = lambda n, s, d=F32: K.sb(n, s, d, st0)
        cw = sb0("cw", [128, 88, 3]); cbias = sb0("cbias", [128, 88]); hflag = sb0("hflag", [128, 1])
        ga2B = sb0("ga2B", [128, D]); gfin = sb0("gfin", [128, D])
        sc.op("sp", lambda e: [e.dma_start(out=cw[:], in_=I["conv_w_t"]), e.dma_start(out=cbias[:], in_=I["conv_b_t"]),
                               e.dma_start(out=hflag[:], in_=I["haloflag"])], writes=["p3_cv"], chan="p3_cv")
        bcast_row(K, ga2B, 5, "ga2B")
        sc.op("sp", lambda e: e.dma_start(out=gfin[:], in_=I["g_final"].partition_broadcast(128)), writes=["gfin"], chan="p3_gfin")
        gact = sb0("gact", [128, 44, 1024], BF16)
        ssf = sb0("ssf", [128, 8, 8]); sst = sb0("sst", [128, 8]); rfin = sb0("rfin", [128, 8])
        wupsrc = I["w_up"].rearrange("(k p) n -> p k n", p=128)
        wdsrc = I["w_down"].rearrange("(i p) n -> p i n", p=128)
        for tb in range(2):
            cb0 = 1024 * tb
            sc.barrier()
            with contextlib.ExitStack() as st:
                sb = lambda n, s, d=F32: K.sb(n, s, d, st)
                ps = lambda n, s, d=F32: K.ps(n, s, d, st)
                h2T = sb("h2Tb", [128, 16, 1026], BF16)
                sc.op("sp", lambda e, cb0=cb0: e.dma_start(out=h2T[:], in_=K.h2T_d[:, :, cb0:cb0 + 1026]), reads=hdeps, writes=["h2Tb"], chan="p3_h2l")
                wup = [[sb(f"wup{vg}{i}", [128, 16, 128], BF16) for i in range(2)] for vg in range(2)]
                upr = [sb(f"upr{vg}", [128, 1026]) for vg in range(2)]
                cv = [sb(f"cv{vg}", [128, 1024]) for vg in range(2)]
                sgl = sb("sgl", [128, 1024])
                psu = [ps(f"p3_psu{i}", [128, 512]) for i in range(3)]
                pn = 0
                for i in range(44):
                    ws = i % 2
                    for vg in range(2):
                        ct = i + 44 * vg
                        sc.op("pool", lambda e, vg=vg, ws=ws, ct=ct: e.dma_start(out=wup[vg][ws][:], in_=wupsrc[:, :, ct * 128:(ct + 1) * 128]),
                              writes=[f"wup{vg}{ws}"], chan=f"p3_wup{vg}{ws}")
                        for (cc0, nn) in ((0, 512), (512, 512), (1024, 2)):
                            pb = pn % 3; pn += 1
                            for k in range(16):
                                sc.op("pe", lambda e, vg=vg, ws=ws, k=k, cc0=cc0, nn=nn, pb=pb: e.matmul(psu[pb][:, 0:nn], lhsT=wup[vg][ws][:, k, :], rhs=h2T[:, k, cc0:cc0 + nn],
                                                                                                       start=(k == 0), stop=(k == 15)),
                                      reads=[f"wup{vg}{ws}", "h2Tb"], writes=[f"p3_psu{pb}"], pe_chain=True)
                            sc.op("act", lambda e, vg=vg, cc0=cc0, nn=nn, pb=pb: e.copy(out=upr[vg][:, cc0:cc0 + nn], in_=psu[pb][:, 0:nn]), reads=[f"p3_psu{pb}"], writes=[f"upr{vg}"])
                        if tb == 0:
                            sc.op("dve", lambda e, vg=vg: e.tensor_scalar(out=upr[vg][:, 0:2], in0=upr[vg][:, 0:2], scalar1=hflag[:, 0:1], scalar2=None, op0=ALU.mult),
                                  reads=[f"upr{vg}", "p3_cv"], writes=[f"upr{vg}"])
                        sc.op("dve", lambda e, vg=vg, ct=ct: e.tensor_scalar(out=cv[vg][:], in0=upr[vg][:, 2:1026], scalar1=cw[:, ct, 2:3], scalar2=cbias[:, ct:ct + 1], op0=ALU.mult, op1=ALU.add),
                              reads=[f"upr{vg}", "p3_cv"], writes=[f"cv{vg}"])
                        sc.op("dve", lambda e, vg=vg, ct=ct: e.scalar_tensor_tensor(out=cv[vg][:], in0=upr[vg][:, 1:1025], scalar=cw[:, ct, 1:2], in1=cv[vg][:], op0=ALU.mult, op1=ALU.add),
                              reads=[f"upr{vg}", "p3_cv", f"cv{vg}"], writes=[f"cv{vg}"])
                        sc.op("dve", lambda e, vg=vg, ct=ct: e.scalar_tensor_tensor(out=cv[vg][:], in0=upr[vg][:, 0:1024], scalar=cw[:, ct, 0:1], in1=cv[vg][:], op0=ALU.mult, op1=ALU.add),
                              reads=[f"upr{vg}", "p3_cv", f"cv{vg}"], writes=[f"cv{vg}"])
                    sc.op("act", lambda e: e.activation(out=sgl[:], in_=cv[1][:], func=AF.Silu), reads=["cv1"], writes=["sgl"])
                    sc.op("pool", lambda e, i=i: e.tensor_tensor(out=gact[:, i, :], in0=sgl[:], in1=cv[0][:], op=ALU.mult), reads=["sgl", "cv0"], writes=["gact"])
            sc.barrier()
            with contextlib.ExitStack() as st:
                sb = lambda n, s, d=F32: K.sb(n, s, d, st)
                ps = lambda n, s, d=F32: K.ps(n, s, d, st)
                wd = [sb(f"wd{i}", [128, 44, 256], BF16) for i in range(2)]
                xmt = [sb(f"xmt{i}", [128, 256]) for i in range(2)]
                xo = [sb(f"xo{i}", [128, 256]) for i in range(2)]
                junk = sb("junk3", [128, 256], BF16)
                psd = [ps(f"p3_psd{i}", [128, 512]) for i in range(2)]
                xfull = [sb(f"xfull{i}", [128, D]) for i in range(2)]
                n2 = 0
                for dh in range(8):
                    ws = dh % 2
                    dc = slice(dh * 256, (dh + 1) * 256)
                    sc.op("pool", lambda e, ws=ws, dc=dc: e.dma_start(out=wd[ws][:], in_=wdsrc[:, :, dc]), writes=[f"wd{ws}"], chan=f"p3_wd{ws}")
                    for tt in range(8):
                        pb = n2 % 2; n2 += 1
                        row0 = 2 + 1024 * tb + tt * 128
                        orow = 1024 * tb + tt * 128
                        for i in range(44):
                            sc.op("pe", lambda e, i=i, tt=tt, ws=ws, pb=pb: e.matmul(psd[pb][:, 0:256], lhsT=gact[:, i, tt * 128:(tt + 1) * 128], rhs=wd[ws][:, i, :],
                                                                                   start=(i == 0), stop=(i == 43)),
                                  reads=["gact", f"wd{ws}"], writes=[f"p3_psd{pb}"], pe_chain=True)
                        sc.op("sp", lambda e, pb=pb, row0=row0, dc=dc: e.dma_start(out=xmt[pb][:], in_=K.xm_d[row0:row0 + 128, dc]), reads=xdeps, writes=[f"xmt{pb}"], chan=f"p3_xmt{pb}")
                        sc.op("dve", lambda e, pb=pb, dc=dc: e.tensor_tensor(out=xo[pb][:], in0=psd[pb][:, 0:256], in1=ga2B[:, dc], op=ALU.mult), reads=[f"p3_psd{pb}", "ga2B"], writes=[f"xo{pb}"])
                        sc.op("dve", lambda e, pb=pb: e.tensor_tensor(out=xo[pb][:], in0=xo[pb][:], in1=xmt[pb][:], op=ALU.add), reads=[f"xo{pb}", f"xmt{pb}"], writes=[f"xo{pb}"])
                        sc.op("act", lambda e, pb=pb, tt=tt, dh=dh: e.activation(out=junk[:], in_=xo[pb][:], func=AF.Square, accum_out=ssf[:, tt, dh:dh + 1]), reads=[f"xo{pb}"], writes=["junk3", "ssf"])
                        sc.op("sp", lambda e, pb=pb, orow=orow, dc=dc: e.dma_start(out=K.xo_d[orow:orow + 128, dc], in_=xo[pb][:]), reads=[f"xo{pb}"], writes=[("xo_d", orow, dh)], chan=f"p3_xos{pb}")
                sc.op("dve", lambda e: e.tensor_reduce(out=sst[:], in_=ssf[:], axis=mybir.AxisListType.X, op=ALU.add), reads=["ssf"], writes=["sst"])
                sc.op("act", lambda e: e.activation(out=rfin[:], in_=sst[:], func=AF.Ln, scale=1.0 / D, bias=EPS), reads=["sst"], writes=["rfin"])
                sc.op("act", lambda e: e.activation(out=rfin[:], in_=rfin[:], func=AF.Exp, scale=-0.5), reads=["rfin"], writes=["rfin"])
                for tt in range(8):
                    fb = tt % 2
                    orow = 1024 * tb + tt * 128
                    sc.op("sp", lambda e, fb=fb, orow=orow: e.dma_start(out=xfull[fb][:], in_=K.xo_d[orow:orow + 128, :]), reads=[("xo_d", orow, dh) for dh in range(8)], writes=[f"xfull{fb}"], chan=f"p3_xfl{fb}")
                    sc.op("dve", lambda e, fb=fb, tt=tt: e.scalar_tensor_tensor(out=xfull[fb][:], in0=xfull[fb][:], scalar=rfin[:, tt:tt + 1], in1=gfin[:], op0=ALU.mult, op1=ALU.mult),
                          reads=[f"xfull{fb}", "rfin", "gfin"], writes=[f"xfull{fb}"])
                    sc.op("sp", lambda e, fb=fb, orow=orow: e.dma_start(out=K.out[orow:orow + 128, :], in_=xfull[fb][:]), reads=[f"xfull{fb}"], writes=[f"out{fb}"], chan=f"p3_out{fb}")


def host_prep(inp, cores=range(8)):
    f32 = np.float32
    x = np.asarray(inp["x"], f32)
    c = np.asarray(inp["c"], f32)
    w_in = np.asarray(inp["w_in"], f32)[0]
    sh = {}
    sh["w_ada"] = np.ascontiguousarray(inp["w_ada"][0], f32)
    sh["b_ada"] = np.ascontiguousarray(inp["b_ada"][0], f32)
    sh["g_mix"] = np.ascontiguousarray(inp["g_mix_norm"][0], f32)
    sh["w_u"] = np.ascontiguousarray(w_in[:, 0:1024])
    for s in ("k", "v"):
        sh["pe_" + s] = np.ascontiguousarray(inp["pe_" + s][0], f32)
        sh["w1_" + s] = np.ascontiguousarray(inp["w1_" + s][0], f32)
        sh["w2_" + s] = np.ascontiguousarray(inp["w2_" + s][0], f32)
    for a, b_ in (("w_glu", "w_glu"), ("b_glu", "b_glu"), ("g_ssm", "g_ssm_out"), ("g_nsa", "g_nsa_out"), ("w_out", "w_out"),
                  ("g_ffn", "g_ffn_norm"), ("w_up", "w_up"), ("conv_w", "conv_w"), ("conv_b", "conv_b"), ("w_down", "w_down")):
        sh[a] = np.ascontiguousarray(inp[b_][0], f32)
    sh["g_final"] = np.ascontiguousarray(inp["g_final"], f32)
    sh["b_ada_t"] = np.ascontiguousarray(sh["b_ada"].reshape(96, 128).T)
    sh["b_glu_t"] = np.ascontiguousarray(sh["b_glu"].reshape(8, 128).T)
    sh["g_ssm_t"] = np.ascontiguousarray(sh["g_ssm"].reshape(8, 128).T)
    sh["g_nsa_t"] = np.ascontiguousarray(sh["g_nsa"].reshape(8, 128).T)
    sh["conv_w_t"] = np.ascontiguousarray(sh["conv_w"].reshape(3, 88, 128).transpose(2, 1, 0))
    sh["conv_b_t"] = np.ascontiguousarray(sh["conv_b"].reshape(88, 128).T)
    sh["pe_kT"] = np.ascontiguousarray(sh["pe_k"].T)
    sh["pe_vT"] = np.ascontiguousarray(sh["pe_v"].T)
    o_q, o_kc, o_vc, o_ks, o_vs, o_kw, o_vw, o_g = 1024, 2048, 2304, 2560, 2816, 3072, 3328, 3584
    wfm = np.zeros((4, D, 640), f32)
    wtm = np.zeros((4, D, 140), f32)
    for hq in range(4):
        hd = slice(64 * hq, 64 * hq + 64)
        ksw = w_in[:, o_ks:o_ks + 256][:, hd]; kww = w_in[:, o_kw:o_kw + 256][:, hd]
        kcw = w_in[:, o_kc:o_kc + 256][:, hd]; vcw = w_in[:, o_vc:o_vc + 256][:, hd]
        wfm[hq] = np.concatenate([w_in[:, o_q + 256 * hq:o_q + 256 * hq + 256], ksw, ksw, kww, kww, kcw, vcw], axis=1)
        gw = w_in[:, o_g + 12 * hq:o_g + 12 * hq + 12]
        gperm = [g * 3 + br for br in range(3) for g in range(4)]
        wtm[hq] = np.concatenate([w_in[:, o_vs:o_vs + 256][:, hd], w_in[:, o_vw:o_vw + 256][:, hd], gw[:, gperm]], axis=1)
    sh["w_fm"] = wfm
    sh["w_tm"] = wtm

    def st_layout(a):
        a = a.reshape((8, 2, 64) + a.shape[2:])
        return np.ascontiguousarray(np.moveaxis(a, 0, 2).reshape((128, 8) + a.shape[3:]))
    names = ("lam_re_t", "lam_im_t", "log_dt_t", "b_re_t", "b_im_t", "c_re_t", "c_im_t", "d_t")
    acc = {n: [] for n in names}
    for hq in range(4):
        gs = slice(16 * hq, 16 * hq + 16)
        lam_re = np.asarray(inp["lam_re"], f32)[0][gs]; lam_im = np.asarray(inp["lam_im"], f32)[0][gs]
        log_dt = np.asarray(inp["log_dt"], f32)[0][gs]
        b_re = np.asarray(inp["b_re"], f32)[0][gs]; b_im = np.asarray(inp["b_im"], f32)[0][gs]
        c_re = np.asarray(inp["c_re"], f32)[0][gs]; c_im = np.asarray(inp["c_im"], f32)[0][gs]
        d_sk = np.asarray(inp["d_skip"], f32)[0][gs]
        acc["lam_re_t"].append(st_layout(lam_re)); acc["lam_im_t"].append(st_layout(lam_im))
        acc["log_dt_t"].append(st_layout(np.broadcast_to(log_dt[:, None], (16, 64)).copy()))
        acc["b_re_t"].append(st_layout(b_re)); acc["b_im_t"].append(st_layout(b_im))
        for nm, cc in (("c_re_t", c_re), ("c_im_t", c_im)):
            ct = np.zeros((128, 8, 64), f32)
            for k4 in range(8):
                for g2 in range(2):
                    c0 = (k4 % 2) * 32 + g2 * 16
                    ct[g2 * 64:(g2 + 1) * 64, k4, c0:c0 + 16] = cc[2 * k4 + g2].T
            acc[nm].append(ct)
        acc["d_t"].append(np.ascontiguousarray(d_sk.reshape(2, 128).T))
    for n in names:
        sh[n] = np.stack(acc[n], 0)
    sh["ident_bf"] = np.eye(128, dtype=f32).astype(NPBF)
    sh["ident_f"] = np.eye(128, dtype=f32)
    sh["iota256"] = np.broadcast_to(np.arange(256, dtype=f32), (128, 256)).copy()
    sh["ones_col"] = np.ones((128, 1), f32).astype(NPBF)
    Pm = np.zeros((128, 128), f32)
    for hh in range(2):
        for i in range(8):
            Pm[hh * 64 + i + 8, hh * 64 + i] = 1.0
            Pm[hh * 64 + i, hh * 64 + i + 8] = 1.0
    sh["Pm"] = Pm.astype(NPBF)
    kl = np.arange(128)[:, None]; ql = np.arange(128)[None, :]
    sh["causal4"] = np.tile((kl <= ql).astype(f32), (1, 4)).astype(NPBF)
    Wimp = np.zeros((512, 128), f32)
    for jb in range(128):
        for o, wv in ((0, 1.0), (1, 2.0), (2, 2.0), (3, 2.0), (4, 1.0)):
            n = 4 * jb + o
            if n <= 510:
                Wimp[n, jb] = wv
    sh["Wimp"] = np.ascontiguousarray(Wimp.reshape(4, 128, 128).transpose(1, 0, 2)).astype(NPBF)
    Z = np.zeros((128, S), f32)
    Z[np.arange(S) // 64, np.arange(S)] = 1.0
    sh["Zexp"] = Z.astype(NPBF)
    inv = (500000.0 ** (-np.arange(8, dtype=np.float64) / 8))
    maps = []
    for core in cores:
        b, j = core // 4, core % 4
        P0 = 6144 - 2048 * j
        m = dict(sh)
        xc = np.zeros((S, D), f32)
        xc[P0:] = x[b, 0:2048 * (j + 1)]
        m["x_ctx"] = xc
        m["valid"] = np.broadcast_to((np.arange(64) * 128 >= P0).astype(f32), (128, 64)).copy()
        xo = np.zeros((2050, D), f32)
        xo[2:] = x[b, 2048 * j:2048 * j + 2048]
        if j > 0:
            xo[:2] = x[b, 2048 * j - 2:2048 * j]
        m["x_own"] = xo
        m["c_own"] = np.ascontiguousarray(c[b])
        m["c_t"] = np.ascontiguousarray(c[b].reshape(16, 128).T)
        pos = np.maximum(np.arange(S) - P0, 0).astype(np.float64)
        angp = pos[None, :] * inv[:, None]
        rC = np.ones((128, S), f32); rS = np.zeros((128, S), f32)
        for hh in range(2):
            rC[hh * 64:hh * 64 + 8] = np.cos(angp); rC[hh * 64 + 8:hh * 64 + 16] = np.cos(angp)
            rS[hh * 64:hh * 64 + 8] = -np.sin(angp); rS[hh * 64 + 8:hh * 64 + 16] = np.sin(angp)
        m["ropeC"] = rC; m["ropeS"] = rS
        rCk = rC.copy(); rSk = rS.copy()
        rCk[64:] = 1.0; rSk[64:] = 0.0
        m["ropeCk"] = rCk; m["ropeSk"] = rSk
        cm = np.zeros((NQ, 128, 4, 128), f32); wm = np.zeros((NQ, 128, 5, 128), f32); sbias = np.zeros((NQ, 128, 128), f32)
        jb0 = P0 // 64
        for qj in range(NQ):
            qt = QF + qj
            pq = qt * 128 + np.arange(128)
            for cc in range(4):
                n = cc * 128 + np.arange(128)
                cm[qj, :, cc, :] = ((16 * n[:, None] + 31 <= pq[None, :]) & (16 * n[:, None] >= P0) & (n[:, None] <= 510)).astype(f32)
            for w in range(5):
                k = (qt - 4 + w) * 128 + np.arange(128)
                wm[qj, :, w, :] = ((k[:, None] <= pq[None, :]) & (k[:, None] > pq[None, :] - 512) & (k[:, None] >= P0)).astype(f32)
            jb = np.arange(128)[None, :]
            cur = (pq // 64)[:, None]
            forced = ((jb == jb0) | (jb == cur) | (jb == cur - 1)) & (jb >= jb0)
            valid = (jb * 64 <= pq[:, None]) & (jb >= jb0)
            sbias[qj] = np.where(forced, 1e30, np.where(valid, 0.0, -1e30))
        m["cmpmask"] = np.tile(cm, (1, 1, 1, 4)).astype(NPBF)
        m["winmask"] = np.tile(wm, (1, 1, 1, 4)).astype(NPBF)
        m["selbias"] = sbias
        m["blockvalid"] = np.broadcast_to((np.arange(128) >= jb0).astype(f32), (128, 128)).copy()
        m["haloflag"] = np.full((128, 1), 0.0 if j == 0 else 1.0, f32)
        maps.append(m)
    return maps


_CACHE = {}


def kernel(**inputs):
    maps = host_prep(inputs)
    if "nc" not in _CACHE:
        _CACHE["nc"] = build()
    nc, K = _CACHE["nc"]
    maps = [{k: v for k, v in m.items() if k in K.I} for m in maps]
    res = run_bass_kernel_spmd(nc, maps, core_ids=list(range(8)))
    out = np.zeros((2, S, D), np.float32)
    for core in range(8):
        b, j = core // 4, core % 4
        out[b, 2048 * j:2048 * j + 2048] = res.results[core]["out"]
    return out
```
